# Optimizing a Trainium2 kernel written in Bass

```python
import math
import jax, jax.numpy as jnp
from jax import lax
import numpy as np

D_MODEL = 1024
BATCH = 16
SEQ = 2048
DEPTH = 1

HEAD_DIM = 64
D_RWKV = D_MODEL // 2
D_ATTN = D_MODEL - D_RWKV
H_RWKV = D_RWKV // HEAD_DIM
H_ATTN = D_ATTN // HEAD_DIM
DECAY_LORA = 64
ICLR_LORA = 64
GATE_LORA = 128
LNX_EPS = 64e-5
DIL_PATTERNS = ((128, 1), (512, 4), (2048, 16))
NEG_INF = -1e30
N_BUCKETS = 32
MAX_DISTANCE = 2048
N_GROUPS = 4
EXPERTS_PER_GROUP = 8
N_EXPERTS = N_GROUPS * EXPERTS_PER_GROUP
TOP_K = 2
D_EXPERT = 512
MOE_BLOCK = 128
D_PLE = 256
LN_EPS = 1e-5
ALPHA = (2 * DEPTH) ** 0.25
BETA = (8 * DEPTH) ** -0.25

kernel_name = "hymba_rwkv7_dilated_hmoe_deepnorm"


def layer_norm(x, g, b):
    xf = x.astype(jnp.float32)
    mu = jnp.mean(xf, -1, keepdims=True)
    var = jnp.mean(jnp.square(xf - mu), -1, keepdims=True)
    return ((xf - mu) * lax.rsqrt(var + LN_EPS) * g + b).astype(x.dtype)


def time_shift(t):
    return jnp.pad(t, ((0, 0), (1, 0), (0, 0)))[:, :-1]


def wkv7_scan(r, decay, k, v, aa, bb):
    B, S, H, N = r.shape

    def step(state, inp):
        r_t, w_t, k_t, v_t, a_t, b_t = inp
        sa = jnp.einsum('bhvk,bhk->bhv', state, a_t)
        state = (state * w_t[:, :, None, :] + sa[..., None] * b_t[:, :, None, :]
                 + v_t[..., None] * k_t[:, :, None, :])
        y_t = jnp.einsum('bhvk,bhk->bhv', state, r_t)
        return state, y_t

    xs = tuple(jnp.moveaxis(t, 1, 0) for t in (r, decay, k, v, aa, bb))
    state0 = jnp.zeros((B, H, N, N), jnp.float32)
    _, y = lax.scan(step, state0, xs)
    return jnp.moveaxis(y, 0, 1)


def rwkv7_mix(h, r_in, k_in, v_in, mu_rkv, mu_lora, w0, w1, w2, a0, a1, a2, g1, g2,
              k_k, k_a, r_k, lnx_g, lnx_b):
    B, S, _ = h.shape
    f32 = jnp.float32
    shift_mix = lambda t, mu: (t + (time_shift(t) - t) * mu).astype(f32)
    r = shift_mix(r_in, mu_rkv[0])
    k = shift_mix(k_in, mu_rkv[1])
    v = shift_mix(v_in, mu_rkv[2])
    dh = time_shift(h) - h
    xw = h + dh * mu_lora[0]
    xa = h + dh * mu_lora[1]
    xg = h + dh * mu_lora[2]
    w_log = -jax.nn.softplus(-(w0 + jnp.tanh(xw @ w1) @ w2).astype(f32)) - 0.5
    decay = jnp.exp(-jnp.exp(w_log))
    a = jax.nn.sigmoid((a0 + (xa @ a1) @ a2).astype(f32))
    g = (jax.nn.sigmoid(xg @ g1) @ g2).astype(f32)
    heads = lambda t: t.reshape(B, S, H_RWKV, HEAD_DIM)
    kk = heads(k * k_k)
    kk = kk * lax.rsqrt(jnp.maximum(jnp.sum(kk * kk, -1, keepdims=True), 1e-24))
    k = k * (1.0 + (a - 1.0) * k_a)
    r, k, v, a, decay = heads(r), heads(k), heads(v), heads(a), heads(decay)
    y = wkv7_scan(r, decay, k, v, -kk, kk * a)
    mu = jnp.mean(y, -1, keepdims=True)
    var = jnp.mean(jnp.square(y - mu), -1, keepdims=True)
    y = ((y - mu) * lax.rsqrt(var + LNX_EPS)).reshape(B, S, D_RWKV) * lnx_g + lnx_b
    bonus = jnp.sum(r * k * r_k, -1, keepdims=True) * v
    return ((y + bonus.reshape(B, S, D_RWKV)) * g).astype(h.dtype)


def t5_bucket(n):
    exact = N_BUCKETS // 2
    nf = np.maximum(n, 1).astype(np.float32)
    large = exact + (np.log(nf / exact) / math.log(MAX_DISTANCE / exact) * (N_BUCKETS - exact)).astype(np.int32)
    large = np.minimum(large, N_BUCKETS - 1)
    return np.where(n < exact, n, large).astype(np.int32)


def dilated_branch(q, k, v, rel_bias, window, dil):
    B, S, H, Dh = q.shape
    L = S // dil
    blk = window // dil
    nb = -(-L // blk)
    Lp = nb * blk

    def subsample(t):
        t = t.reshape(B, L, dil, H, Dh).transpose(0, 2, 3, 1, 4)
        return jnp.pad(t, ((0, 0), (0, 0), (0, 0), (0, Lp - L), (0, 0)))

    def band(t):
        t = jnp.pad(subsample(t), ((0, 0), (0, 0), (0, 0), (blk, 0), (0, 0))).reshape(B, dil, H, nb + 1, blk, Dh)
        return jnp.concatenate([t[:, :, :, :-1], t[:, :, :, 1:]], axis=4)

    qs = subsample(q).reshape(B, dil, H, nb, blk, Dh)
    kb, vb = band(k), band(v)
    rel = np.arange(blk)[:, None] + blk - np.arange(2 * blk)[None, :]
    kpos = np.arange(nb)[:, None] * blk + np.arange(2 * blk)[None, :] - blk
    valid = ((rel >= 0) & (rel <= blk))[None] & (kpos >= 0)[:, None, :]
    bias = jnp.transpose(rel_bias[t5_bucket(np.clip(rel, 0, None) * dil)], (2, 0, 1)).astype(jnp.float32)
    logits = jnp.einsum('bdhnqc,bdhnkc->bdhnqk', qs, kb) * (HEAD_DIM ** -0.5) + bias[None, None, :, None]
    logits = jnp.where(valid, logits, NEG_INF)
    m = jnp.max(logits, -1, keepdims=True)
    e = jnp.exp(logits - m)
    s = jnp.sum(e, -1, keepdims=True)
    o = jnp.einsum('bdhnqk,bdhnkc->bdhnqc', e, vb) / s
    lse = (m + jnp.log(s))[..., 0]

    def unsample(t):
        t = t.reshape(B, dil, H, Lp, *t.shape[5:])[:, :, :, :L]
        t = jnp.moveaxis(t, 3, 1)
        return t.reshape(B, S, H, *t.shape[4:])

    return unsample(o), unsample(lse)


def dilated_attention(q, k, v, rel_bias):
    outs, lses = [], []
    for window, dil in DIL_PATTERNS:
        o, l = dilated_branch(q, k, v, rel_bias, window, dil)
        outs.append(o)
        lses.append(l)
    wts = jax.nn.softmax(jnp.stack(lses, 0), axis=0)
    return jnp.einsum('pbsh,pbshc->bshc', wts, jnp.stack(outs, 0))


def hier_moe(x2, wr_g, br_g, wr_e, br_e, w_gate, w_up, w_down):
    N, D = x2.shape
    lg = (x2 @ wr_g).astype(jnp.float32) + br_g
    pg = jax.nn.softmax(lg, -1)
    grp = jnp.argmax(lg, -1)
    wg = jnp.take_along_axis(pg, grp[:, None], axis=-1)
    le = ((x2 @ wr_e).astype(jnp.float32) + br_e).reshape(N, N_GROUPS, EXPERTS_PER_GROUP)
    le = le[jnp.arange(N), grp]
    top_v, top_i = lax.top_k(le, TOP_K)
    we = jax.nn.softmax(top_v, -1) * wg
    eid = grp[:, None] * EXPERTS_PER_GROUP + top_i
    A = N * TOP_K
    e_flat = eid.reshape(A).astype(jnp.int32)
    w_flat = we.reshape(A)
    tok_flat = jnp.arange(A, dtype=jnp.int32) // TOP_K
    order = jnp.argsort(e_flat)
    e_sorted = e_flat[order]
    counts = jnp.bincount(e_flat, length=N_EXPERTS)
    start = jnp.cumsum(counts) - counts
    padded = (counts + MOE_BLOCK - 1) // MOE_BLOCK * MOE_BLOCK
    pend = jnp.cumsum(padded)
    pstart = pend - padded
    dest = pstart[e_sorted] + (jnp.arange(A) - start[e_sorted])
    nblk = -(-A // MOE_BLOCK) + N_EXPERTS
    P = nblk * MOE_BLOCK
    row_tok = jnp.zeros((P,), jnp.int32).at[dest].set(tok_flat[order])
    row_w = jnp.zeros((P,), jnp.float32).at[dest].set(w_flat[order])
    blk_e = jnp.minimum(jnp.searchsorted(pend, jnp.arange(nblk) * MOE_BLOCK, side='right'), N_EXPERTS - 1)
    xb = x2[row_tok].reshape(nblk, MOE_BLOCK, D)

    def run_block(args):
        xblk, e = args
        hdn = jax.nn.silu(xblk @ w_gate[e]) * (xblk @ w_up[e])
        return hdn @ w_down[e]

    yb = lax.map(run_block, (xb, blk_e)).reshape(P, D)
    return jnp.zeros((N, D), x2.dtype).at[row_tok].add((yb * row_w[:, None]).astype(x2.dtype))


def setup_inputs(seed: int = 0) -> dict:
    key = jax.random.key(seed)
    ks = iter(jax.random.split(key, 48))
    nrm = lambda shape, scale: jax.random.normal(next(ks), shape, jnp.float32) * scale
    L = DEPTH
    ramp = jnp.arange(D_RWKV, dtype=jnp.float32) / (D_RWKV - 1)
    d_in = 3 * D_RWKV + 3 * D_ATTN
    return {
        "x": nrm((BATCH, SEQ, D_MODEL), 1.0),
        "p": nrm((DEPTH, BATCH, SEQ, D_PLE), 1.0),
        "w_in": nrm((L, D_MODEL, d_in), D_MODEL ** -0.5),
        "mu_rkv": jax.random.uniform(next(ks), (L, 3, D_RWKV), jnp.float32),
        "mu_lora": jax.random.uniform(next(ks), (L, 3, D_MODEL), jnp.float32),
        "w0": (-6.5 + 5.0 * ramp ** 0.85)[None] + nrm((L, D_RWKV), 0.1),
        "w_lora1": nrm((L, D_MODEL, DECAY_LORA), D_MODEL ** -0.5),
        "w_lora2": nrm((L, DECAY_LORA, D_RWKV), 0.5 * DECAY_LORA ** -0.5),
        "a0": nrm((L, D_RWKV), 0.1),
        "a_lora1": nrm((L, D_MODEL, ICLR_LORA), D_MODEL ** -0.5),
        "a_lora2": nrm((L, ICLR_LORA, D_RWKV), ICLR_LORA ** -0.5),
        "g_lora1": nrm((L, D_MODEL, GATE_LORA), D_MODEL ** -0.5),
        "g_lora2": nrm((L, GATE_LORA, D_RWKV), GATE_LORA ** -0.5),
        "k_k": 0.85 + nrm((L, D_RWKV), 0.02),
        "k_a": 1.0 + nrm((L, D_RWKV), 0.02),
        "r_k": nrm((L, H_RWKV, HEAD_DIM), 0.1),
        "lnx_g": 1.0 + nrm((L, D_RWKV), 0.02),
        "lnx_b": nrm((L, D_RWKV), 0.02),
        "rel_bias": nrm((N_BUCKETS, H_ATTN), 0.5),
        "w_o": nrm((L, D_MODEL, D_MODEL), BETA * D_MODEL ** -0.5),
        "ln1_g": 1.0 + nrm((L, D_MODEL), 0.02),
        "ln1_b": nrm((L, D_MODEL), 0.02),
        "router_g": nrm((L, D_MODEL, N_GROUPS), D_MODEL ** -0.5),
        "router_g_b": nrm((L, N_GROUPS), 0.01),
        "router_e": nrm((L, D_MODEL, N_EXPERTS), D_MODEL ** -0.5),
        "router_e_b": nrm((L, N_EXPERTS), 0.01),
        "w_gate": nrm((L, N_EXPERTS, D_MODEL, D_EXPERT), D_MODEL ** -0.5),
        "w_up": nrm((L, N_EXPERTS, D_MODEL, D_EXPERT), D_MODEL ** -0.5),
        "w_down": nrm((L, N_EXPERTS, D_EXPERT, D_MODEL), BETA * D_EXPERT ** -0.5),
        "ple_gate": nrm((L, D_MODEL, D_MODEL), D_MODEL ** -0.5),
        "ple_proj": nrm((L, D_PLE, D_MODEL), BETA * D_PLE ** -0.5),
        "ln2_g": 1.0 + nrm((L, D_MODEL), 0.02),
        "ln2_b": nrm((L, D_MODEL), 0.02),
    }


def reference(x, p, w_in, mu_rkv, mu_lora, w0, w_lora1, w_lora2, a0, a_lora1, a_lora2,
              g_lora1, g_lora2, k_k, k_a, r_k, lnx_g, lnx_b, rel_bias, w_o, ln1_g, ln1_b,
              router_g, router_g_b, router_e, router_e_b, w_gate, w_up, w_down,
              ple_gate, ple_proj, ln2_g, ln2_b):
    B, S, D = x.shape
    splits = [D_RWKV, 2 * D_RWKV, 3 * D_RWKV, 3 * D_RWKV + D_ATTN, 3 * D_RWKV + 2 * D_ATTN]
    for i in range(DEPTH):
        h = x
        proj = h @ w_in[i]
        r_in, k_in, v_in, q_at, k_at, v_at = jnp.split(proj, splits, axis=-1)
        y_rwkv = rwkv7_mix(h, r_in, k_in, v_in, mu_rkv[i], mu_lora[i], w0[i], w_lora1[i], w_lora2[i],
                           a0[i], a_lora1[i], a_lora2[i], g_lora1[i], g_lora2[i],
                           k_k[i], k_a[i], r_k[i], lnx_g[i], lnx_b[i])
        to_heads = lambda t: t.astype(jnp.float32).reshape(B, S, H_ATTN, HEAD_DIM)
        y_att = dilated_attention(to_heads(q_at), to_heads(k_at), to_heads(v_at), rel_bias)
        y_att = y_att.reshape(B, S, D_ATTN).astype(x.dtype)
        mix = jnp.concatenate([y_rwkv, y_att], axis=-1) @ w_o[i]
        x = layer_norm(ALPHA * x + mix, ln1_g[i], ln1_b[i])
        moe = hier_moe(x.reshape(B * S, D), router_g[i], router_g_b[i], router_e[i], router_e_b[i],
                       w_gate[i], w_up[i], w_down[i]).reshape(B, S, D)
        ple = jax.nn.sigmoid(x @ ple_gate[i]) * (p[i] @ ple_proj[i])
        x = layer_norm(ALPHA * x + moe + ple, ln2_g[i], ln2_b[i])
    return x
```

```python
import numpy as np
import os
from contextlib import ExitStack
import concourse.bass as bass
import concourse.mybir as mybir
from concourse.bass_utils import run_bass_kernel_spmd

F32 = mybir.dt.float32
BF16 = mybir.dt.bfloat16
AF = mybir.ActivationFunctionType
ALU = mybir.AluOpType
AX = mybir.AxisListType

ENGS = ("pe", "dve", "act", "pool", "sp")
NB = 2
S = 2048
D = 1024
NEG = -30000.0
ALPHA = 2.0 ** 0.25
LN_EPS = 1e-5
LNX_EPS = 64e-5


class KB:
    def __init__(self, nc, n_dma_sems=48):
        self.nc = nc
        self.es = ExitStack()
        self.lists = {e: [] for e in ENGS}
        self.esem = {e: self.es.enter_context(nc.semaphore("s_" + e)) for e in ENGS if e != "sp"}
        self.ecnt = {e: 0 for e in ENGS}
        self.dsems = [self.es.enter_context(nc.semaphore("d%d" % i)) for i in range(n_dma_sems)]
        self.dcnt = [0] * n_dma_sems
        self.dnext = 0
        self.dnext_sw = 0
        self.waited = {e: {} for e in ENGS}
        self.lastw = {}
        self.readers = {}
        self.semobj = {}
        for e, s in self.esem.items():
            self.semobj[("e", e)] = s
        for i, s in enumerate(self.dsems):
            self.semobj[("d", i)] = s
        self.pes = None
        self.uid = 0
        self.last_func = None
        self.scr = None

    def phase_begin(self):
        self.pes = ExitStack()

    def sb(self, name, shape, dt, persistent=False):
        self.uid += 1
        st = self.es if (persistent or self.pes is None) else self.pes
        return st.enter_context(self.nc.sbuf_tensor("%s_%d" % (name, self.uid), list(shape), dt))

    def ps(self, name, shape, dt=F32):
        self.uid += 1
        return self.pes.enter_context(self.nc.psum_tensor("%s_%d" % (name, self.uid), list(shape), dt))

    def _need(self, eng, ev):
        if ev is None:
            return
        key, val = ev
        if eng == "pe" and key == ("e", "pe"):
            return
        if self.waited[eng].get(key, 0) >= val:
            return
        self.waited[eng][key] = val
        self.lists[eng].append(("wait", key, val))

    def _deps(self, eng, reads, writes):
        if os.environ.get("DBG_SWAP"):
            for w in writes:
                self._need(eng, self.lastw.get(w))
                for ev in self.readers.get(w, ()):
                    self._need(eng, ev)
        for r in reads:
            self._need(eng, self.lastw.get(r))
        for w in writes:
            self._need(eng, self.lastw.get(w))
            for ev in self.readers.get(w, ()):
                self._need(eng, ev)

    def _commit(self, ev, reads, writes):
        for r in reads:
            self.readers.setdefault(r, []).append(ev)
        for w in writes:
            self.lastw[w] = ev
            self.readers[w] = []

    def op(self, eng, fn, r=(), w=()):
        psr = [k for k in r if k.startswith("ps")]
        if psr:
            r = [k for k in r if not k.startswith("ps")]
            w = list(w) + psr
        self._deps(eng, r, w)
        self.ecnt[eng] += 1
        ev = (("e", eng), self.ecnt[eng])
        self.lists[eng].append(("op", fn, ("e", eng)))
        self._commit(ev, r, w)
        return ev

    def dma(self, q, out, in_, r=(), w=(), slow=False):
        self._deps(q, r, w)
        nsw = 8
        if q == "pool":
            i = self.dnext_sw
            self.dnext_sw = (self.dnext_sw + 1) % nsw
        else:
            i = nsw + self.dnext
            self.dnext = (self.dnext + 1) % (len(self.dsems) - nsw)
        if self.dcnt[i] > 0:
            self._need(q, (("d", i), 16 * self.dcnt[i]))
        self.dcnt[i] += 1
        ev = (("d", i), 16 * self.dcnt[i])
        if slow:
            fn = lambda e: e.dma_start(out=out, in_=in_, allow_slow_non_contiguous=True)
        else:
            fn = lambda e: e.dma_start(out=out, in_=in_)
        self.lists[q].append(("dma", fn, ("d", i)))
        self._commit(ev, r, w)
        return ev

    def barrier(self):
        evs = set(self.lastw.values())
        for rs in self.readers.values():
            evs.update(rs)
        for eng in ENGS:
            for ev in evs:
                self._need(eng, ev)
        self.lastw = {}
        self.readers = {}

    def sync_all(self):
        if os.environ.get("KEEP_BARRIERS"):
            self.barrier()
            self.emit()

    def phase_end(self):
        self.barrier()
        self.emit()
        self.pes.close()
        self.pes = None

    def emit(self):
        nc = self.nc
        semobj = self.semobj
        lists = self.lists

        nopmode = os.environ.get("DBG_NOP", "")

        attach = os.environ.get("DBG_ATTACH", "1") == "1"

        def run(engobj, items):
            pend = []
            for it in items:
                if it[0] == "wait":
                    pend.append(it)
                    continue
                last = None
                if attach and pend:
                    last = pend.pop()
                for w_ in pend:
                    engobj.wait_ge(semobj[w_[1]], w_[2])
                pend = []
                ins = it[1](engobj)
                if it[0] == "raw":
                    continue
                if last is not None:
                    ins = ins._wait_ge(semobj[last[1]], last[2])
                ins.then_inc(semobj[it[2]], 1 if it[0] == "op" else 16)
            for w_ in pend:
                engobj.wait_ge(semobj[w_[1]], w_[2])

        with nc.Block() as block:
            @block.tensor
            def _(e):
                run(e, lists["pe"])

            @block.vector
            def _(e):
                run(e, lists["dve"])

            @block.scalar
            def _(e):
                run(e, lists["act"])

            @block.gpsimd
            def _(e):
                run(e, lists["pool"])

            @block.sync
            def _(e):
                run(e, lists["sp"])
        self.lists = {e: [] for e in ENGS}

    def close(self):
        self.es.close()

    def mm(self, out, lhsT, rhs, start=True, stop=True, r=(), w=()):
        return self.op("pe", lambda e: e.matmul(out, lhsT=lhsT, rhs=rhs, start=start, stop=stop), r, w)

    def tr(self, out, in_, ident, r=(), w=()):
        return self.op("pe", lambda e: e.transpose(out, in_, ident), r, w)

    def act(self, out, in_, func, bias=None, scale=None, r=(), w=()):
        kw = {}
        if bias is not None:
            kw["bias"] = bias
        if scale is not None:
            kw["scale"] = scale
        return self.op("act", lambda e: e.activation(out, in_, func, **kw), r, w)

    def tt(self, eng, out, a, b, op, r=(), w=()):
        return self.op(eng, lambda e: e.tensor_tensor(out, a, b, op), r, w)

    def ts(self, eng, out, a, s1, s2, op0, op1=None, r=(), w=()):
        if op1 is None:
            return self.op(eng, lambda e: e.tensor_scalar(out, a, s1, None, op0), r, w)
        return self.op(eng, lambda e: e.tensor_scalar(out, a, s1, s2, op0, op1), r, w)

    def stt(self, eng, out, a, s, b, op0, op1, r=(), w=()):
        return self.op(eng, lambda e: e.scalar_tensor_tensor(out, a, s, b, op0, op1), r, w)

    def cp(self, eng, out, in_, r=(), w=()):
        if eng == "act":
            return self.op("act", lambda e: e.copy(out, in_), r, w)
        return self.op(eng, lambda e: e.tensor_copy(out, in_), r, w)

    def memset(self, eng, ap, v, r=(), w=()):
        return self.op(eng, lambda e: e.memset(ap, v), r, w)


def t5_bucket(n):
    import math
    exact = 16
    nf = np.maximum(n, 1).astype(np.float32)
    large = exact + (np.log(nf / exact) / math.log(2048 / exact) * (32 - exact)).astype(np.int32)
    large = np.minimum(large, 31)
    return np.where(n < exact, n, large).astype(np.int32)


DILS = (1, 4, 16)


def bias_index_tables():
    kj = np.arange(128)[:, None]
    qi = np.arange(128)[None, :]
    out = np.zeros((3, 128, 256), np.int64)
    for p, dil in enumerate(DILS):
        rel_c = qi - kj
        cur = np.where(rel_c >= 0, t5_bucket(np.clip(rel_c, 0, None) * dil), 32)
        rel_p = qi + 128 - kj
        prv = np.where(rel_p <= 128, t5_bucket(np.clip(rel_p, 0, None) * dil), 32)
        out[p, :, :128] = cur
        out[p, :, 128:] = prv
    return out


class Prog:
    pass


def build_program(dbg=None, stop_after=None, nb=NB):
    nc = bass.Bass("TRN2", target_bir_lowering=False)
    P = Prog()
    dt = lambda n, s, d=F32, k="ExternalInput": nc.dram_tensor(n, list(s), d, kind=k).ap()
    x = dt("x", [nb, S, D])
    pin = dt("p", [nb, S, 256])
    w_in = dt("w_in", [D, 3072])
    mu_rkv = dt("mu_rkv", [1536])
    mu_lora = dt("mu_lora", [3, D])
    w0 = dt("w0", [512]); a0 = dt("a0", [512])
    w_l1 = dt("w_lora1", [D, 64]); w_l2 = dt("w_lora2", [64, 512])
    a_l1 = dt("a_lora1", [D, 64]); a_l2 = dt("a_lora2", [64, 512])
    g_l1 = dt("g_lora1", [D, 128]); g_l2 = dt("g_lora2", [128, 512])
    k_k = dt("k_k", [512]); k_a = dt("k_a", [512]); r_k = dt("r_k", [512])
    lnx_g = dt("lnx_g", [512]); lnx_b = dt("lnx_b", [512])
    biasT = dt("biasT", [3, 8, 128, 256])
    w_o = dt("w_o", [D, D])
    ln1_g = dt("ln1_g", [D]); ln1_b = dt("ln1_b", [D])
    router = dt("router", [D, 36]); router_b = dt("router_b", [36])
    w_gate = dt("w_gate", [32, D, 512]); w_up = dt("w_up", [32, D, 512]); w_down = dt("w_down", [32, 512, D])
    ple_gate = dt("ple_gate", [D, D]); ple_proj = dt("ple_proj", [256, D])
    ln2_g = dt("ln2_g", [D]); ln2_b = dt("ln2_b", [D])
    out = dt("out", [nb, S, D], F32, "ExternalOutput")
    dbg_out = None
    if dbg is not None:
        dbg_out = dt("dbg", dbg[1], F32, "ExternalOutput")
    Wa = dt("Wa", [D, 3072], BF16, "Internal")
    Wb = dt("Wb", [D, 1536], BF16, "Internal")
    x1D = dt("x1D", [nb, S, D], F32, "Internal")
    mixD = dt("mixD", [nb, D, S], BF16, "Internal")
    x1TD = dt("x1TD", [nb, D, S], BF16, "Internal")
    gateD = dt("gateD", [nb, S, 32], F32, "Internal")

    kb = KB(nc)
    dq = ["sp", "sp"] if os.environ.get("DBG_SPONLY") else ["sp", "act"]
    dqi = [0]

    def q():
        dqi[0] ^= 1
        return dq[dqi[0]]

    identF = kb.sb("identF", [128, 128], F32, True)
    identB = kb.sb("identB", [128, 128], BF16, True)
    onesB = kb.sb("onesB", [128, 64], BF16, True)
    ones64 = kb.sb("ones64", [64, 64], F32, True)
    mean64 = kb.sb("mean64", [64, 64], F32, True)
    mSU = kb.sb("mSU", [128, 128], F32, True)
    mSL = kb.sb("mSL", [128, 128], F32, True)
    mIU = kb.sb("mIU", [128, 128], F32, True)
    m2SU = kb.sb("m2SU", [128, 256], F32, True)
    m2IU = kb.sb("m2IU", [128, 256], F32, True)
    BDones = kb.sb("BDones", [128, 128], F32, True)
    BDmean = kb.sb("BDmean", [128, 128], F32, True)
    csm = kb.sb("csm", [64, 256], F32, True)
    ones128 = kb.sb("ones128", [128, 128], F32, True)
    L1a = kb.sb("L1a", [128, 8, 256], BF16, True)
    L1b = kb.sb("L1b", [128, 8, 256], BF16, True)
    W2A2 = kb.sb("W2A2", [128, 512], BF16, True)
    G2 = kb.sb("G2", [128, 512], BF16, True)
    hv = kb.sb("hv", [128, 9, 4], F32, True)
    (V_W0, V_A0, V_KK, V_KA, V_RK, V_LG, V_LB, V_1MKA, V_X) = range(9)

    kb.scr = kb.sb("actscr", [128, 2], F32, True)
    kb.phase_begin()
    kb.memset("pool", kb.scr[:], 0.5, w=["actscr"])
    kb.memset("pool", identF[:], 0.0, w=["identF"])
    kb.op("pool", lambda e: e.affine_select(identF[:], identF[:], [[-1, 128]], ALU.not_equal, 1.0, base=0, channel_multiplier=1),
          r=["identF"], w=["identF"])
    kb.cp("dve", identB[:], identF[:], r=["identF"], w=["identB"])
    kb.memset("pool", onesB[:], 1.0, w=["onesB"])
    kb.memset("pool", ones64[:], 1.0, w=["ones64"])
    kb.memset("pool", mean64[:], 1.0 / 64.0, w=["mean64"])
    kb.memset("pool", mSU[:], 1.0, w=["mSU"])
    kb.op("pool", lambda e: e.affine_select(mSU[:], mSU[:], [[1, 128]], ALU.is_gt, 0.0, base=0, channel_multiplier=-1), r=["mSU"], w=["mSU"])
    kb.memset("pool", mSL[:], 1.0, w=["mSL"])
    kb.op("pool", lambda e: e.affine_select(mSL[:], mSL[:], [[-1, 128]], ALU.is_gt, 0.0, base=0, channel_multiplier=1), r=["mSL"], w=["mSL"])
    kb.memset("pool", mIU[:], 1.0, w=["mIU"])
    kb.op("pool", lambda e: e.affine_select(mIU[:], mIU[:], [[1, 128]], ALU.is_ge, 0.0, base=0, channel_multiplier=-1), r=["mIU"], w=["mIU"])
    kb.cp("pool", m2SU[:, 0:128], mSU[:], r=["mSU"], w=["m2SU"])
    kb.cp("pool", m2SU[:, 128:256], mSU[:], r=["mSU"], w=["m2SU"])
    kb.cp("pool", m2IU[:, 0:128], mIU[:], r=["mIU"], w=["m2IU"])
    kb.cp("pool", m2IU[:, 128:256], mIU[:], r=["mIU"], w=["m2IU"])
    for t_, val in ((BDones, 1.0), (BDmean, 1.0 / 64.0)):
        kb.memset("pool", t_[:], 0.0, w=["BD"])
        kb.memset("pool", t_[0:64, 0:64], val, r=["BD"], w=["BD"])
        kb.memset("pool", t_[64:128, 64:128], val, r=["BD"], w=["BD"])
    kb.memset("pool", csm[:], 0.0, w=["csm"])
    kb.memset("pool", ones128[:], 1.0, w=["ones128"])
    kb.ts("dve", csm[:, 0:64], identF[0:64, 0:64], -1.0 / 64.0, None, ALU.add, r=["identF", "csm"], w=["csm"])
    kb.ts("dve", csm[:, 192:256], identF[0:64, 0:64], -1.0 / 64.0, None, ALU.add, r=["identF", "csm"], w=["csm"])
    for i, v in ((V_W0, w0), (V_A0, a0), (V_KK, k_k), (V_KA, k_a), (V_RK, r_k), (V_LG, lnx_g), (V_LB, lnx_b)):
        kb.dma("sp", hv[:, i, :], v.rearrange("(h p) -> p h", p=128), w=["hv"], slow=True)
    kb.ts("dve", hv[:, V_1MKA, :], hv[:, V_KA, :], -1.0, 1.0, ALU.mult, ALU.add, r=["hv"], w=["hv"])
    kb.dma("pool", W2A2[0:64, :], w_l2[:, :], w=["W2A2"])
    kb.dma("pool", W2A2[64:128, :], a_l2[:, :], w=["W2A2"])
    kb.dma("pool", G2[:], g_l2[:, :], w=["G2"])
    mul = kb.sb("mul", [128, 3, 8], F32)
    omul = kb.sb("omul", [128, 3, 8], F32)
    for j in range(3):
        kb.dma("sp", mul[:, j, :], mu_lora[j].rearrange("(c p) -> p c", p=128), w=["mul"], slow=True)
    kb.ts("dve", omul[:], mul[:], -1.0, 1.0, ALU.mult, ALU.add, r=["mul"], w=["omul"])
    l1t = kb.sb("l1t", [128, 8, 256], F32)
    kb.dma("sp", l1t[:, :, 0:64], w_l1.rearrange("(c p) j -> p c j", p=128), w=["l1t"])
    kb.dma(q(), l1t[:, :, 64:128], a_l1.rearrange("(c p) j -> p c j", p=128), w=["l1t"])
    kb.dma("sp", l1t[:, :, 128:256], g_l1.rearrange("(c p) j -> p c j", p=128), w=["l1t"])
    for c in range(8):
        for j, (lo, hi) in enumerate(((0, 64), (64, 128), (128, 256))):
            kb.ts("dve", L1a[:, c, lo:hi], l1t[:, c, lo:hi], omul[:, j, c:c + 1], None, ALU.mult, r=["l1t", "omul"], w=["L1a"])
            kb.ts("pool", L1b[:, c, lo:hi], l1t[:, c, lo:hi], mul[:, j, c:c + 1], None, ALU.mult, r=["l1t", "mul"], w=["L1b"])
    mubc = kb.sb("mubc", [128, 1536], F32)
    omubc = kb.sb("omubc", [128, 1536], F32)
    kb.dma("sp", mubc[:], mu_rkv.partition_broadcast(128), w=["mubc"])
    kb.ts("dve", omubc[:], mubc[:], -1.0, 1.0, ALU.mult, ALU.add, r=["mubc"], w=["omubc"])
    wt = [kb.sb("wt%d" % i, [128, 3072], F32) for i in range(2)]
    wa = [kb.sb("wa%d" % i, [128, 3072], BF16) for i in range(2)]
    wb = [kb.sb("wb%d" % i, [128, 1536], BF16) for i in range(2)]
    for c in range(8):
        i = c % 2
        kb.dma(q(), wt[i][:], w_in[c * 128:(c + 1) * 128, :], w=["wt%d" % i])
        kb.tt("dve", wa[i][:, 0:1536], wt[i][:, 0:1536], omubc[:], ALU.mult, r=["wt%d" % i, "omubc"], w=["wa%d" % i])
        kb.cp("act", wa[i][:, 1536:3072], wt[i][:, 1536:3072], r=["wt%d" % i], w=["wa%d" % i])
        kb.tt("pool", wb[i][:], wt[i][:, 0:1536], mubc[:], ALU.mult, r=["wt%d" % i, "mubc"], w=["wb%d" % i])
        kb.dma(q(), Wa[c * 128:(c + 1) * 128, :], wa[i][:], r=["wa%d" % i], w=["WaD"])
        kb.dma(q(), Wb[c * 128:(c + 1) * 128, :], wb[i][:], r=["wb%d" % i], w=["WbD"])
    kb.phase_end()

    def dump(ap_sb, key, dram_ap):
        kb.dma("sp", dram_ap, ap_sb, r=[key], w=["dbgout"])

    for b in range(nb):
        bes = ExitStack()
        hT = bes.enter_context(nc.sbuf_tensor("hT_b%d" % b, [128, 8, S + 1], BF16))

        kb.phase_begin()
        l1wa = bes.enter_context(nc.sbuf_tensor("l1wa_b%d" % b, [128, S], BF16))
        l1g = bes.enter_context(nc.sbuf_tensor("l1g_b%d" % b, [128, S], BF16))
        xt = [kb.sb("xt%d" % i, [128, D], F32) for i in range(2)]
        psA = [kb.ps("psA%d" % i, [128, 512]) for i in range(4)]
        kb.memset("pool", hT[:, :, 0:1], 0.0, w=["hT"])
        pi = 0
        for t in range(16):
            i = t % 2
            kb.dma(q(), xt[i][:], x[b, t * 128:(t + 1) * 128, :], w=["xt%d" % i])
            for half in range(2):
                pk = "psA%d" % (pi % 4)
                ps = psA[pi % 4]
                pi += 1
                for cc in range(4):
                    c = half * 4 + cc
                    kb.tr(ps[:, cc * 128:(cc + 1) * 128], xt[i][:, c * 128:(c + 1) * 128], identF[:],
                          r=["xt%d" % i, "identF"], w=[pk])
                eng = "dve" if half == 0 else "act"
                kb.cp(eng, hT[:, half * 4:half * 4 + 4, 1 + t * 128:1 + (t + 1) * 128],
                      ps[:].rearrange("p (c t) -> p c t", t=128), r=[pk], w=["hT"])
        for tt_ in range(4):
            ts_ = slice(tt_ * 512, (tt_ + 1) * 512)
            for oc in range(2):
                pk = "psA%d" % (pi % 4)
                ps = psA[pi % 4]
                pi += 1
                for c in range(8):
                    kb.mm(ps[:], L1a[:, c, oc * 128:(oc + 1) * 128], hT[:, c, 1 + tt_ * 512:1 + (tt_ + 1) * 512],
                          start=(c == 0), stop=False, r=["hT", "L1a"], w=[pk])
                    kb.mm(ps[:], L1b[:, c, oc * 128:(oc + 1) * 128], hT[:, c, tt_ * 512:(tt_ + 1) * 512],
                          start=False, stop=(c == 7), r=["hT", "L1b"], w=[pk])
                if oc == 0:
                    kb.act(l1wa[0:64, ts_], ps[0:64, :], AF.Tanh, r=[pk], w=["l1wa"])
                    kb.cp("dve", l1wa[64:128, ts_], ps[64:128, :], r=[pk], w=["l1wa"])
                else:
                    kb.act(l1g[:, ts_], ps[:], AF.Sigmoid, r=[pk], w=["l1g"])
        if dbg and dbg[0] == "hT" and b == 0:
            tmp = kb.sb("dbgtmp", [128, S], F32)
            kb.cp("dve", tmp[:], hT[:, 3, 1:S + 1], r=["hT"], w=["dbgtmp"])
            dump(tmp[:], "dbgtmp", dbg_out[:, :])
        kb.phase_end()
        if stop_after == "A":
            bes.close()
            break

        for hg in range(0 if os.environ.get('DBG_SKIPB') else 2):
            kb.phase_begin()
            QT = kb.sb("QT", [128, 2, S], BF16)
            KT = kb.sb("KT", [128, 2, S], BF16)
            V3 = kb.sb("V3", [128, 3, 16, 4, 65], BF16)
            kb.memset("pool", V3[:, :, :, :, 64:65], 1.0, w=["V3"])
            wq = kb.sb("wq", [128, 8, 768], BF16)
            numA = kb.sb("numA", [65, S], F32)
            NL = 5
            PT = [kb.sb("PT%d" % i, [128, 256], BF16) for i in range(NL)]
            yat = [kb.sb("yat%d" % i, [64, S], BF16) for i in range(2)]
            biasS = kb.sb("biasS", [128, 24, 256], BF16)
            kb.dma("pool", biasS[:], biasT.rearrange("p h k q -> k (p h) q"), w=["biasS"])
            pbk = [kb.ps("psK%d" % i, [128, 512]) for i in range(8)]
            psP = pbk[0:2]
            psL = pbk[0:NL]
            psN = pbk[NL:8]
            for j, off in enumerate((1536, 2048, 2560)):
                kb.dma(q(), wq[:, :, j * 256:(j + 1) * 256],
                       Wa[:, off + hg * 256: off + (hg + 1) * 256].rearrange("(c p) f -> p c f", p=128),
                       r=["WaD"], w=["wq"])
            pi = 0
            for j, dst in ((0, QT), (1, KT)):
                for fc in range(2):
                    for tt_ in range(4):
                        pk = "psK%d" % (pi % 2)
                        ps = psP[pi % 2]
                        pi += 1
                        for c in range(8):
                            kb.mm(ps[:], wq[:, c, j * 256 + fc * 128: j * 256 + (fc + 1) * 128],
                                  hT[:, c, 1 + tt_ * 512:1 + (tt_ + 1) * 512], start=(c == 0), stop=(c == 7),
                                  r=["hT", "wq"], w=[pk])
                        if j == 0:
                            kb.act(dst[:, fc, tt_ * 512:(tt_ + 1) * 512], ps[:], AF.Copy, scale=0.125, r=[pk], w=["QT"])
                        else:
                            kb.cp("dve", dst[:, fc, tt_ * 512:(tt_ + 1) * 512], ps[:], r=[pk], w=["KT"])
            for p_, dil in enumerate(DILS):
                for ti in range(16):
                    nbk = 16 // dil
                    rcls, m = ti // nbk, ti % nbk
                    pk = "psK%d" % (pi % 2)
                    ps = psP[pi % 2]
                    pi += 1
                    st = 1 + dil * 128 * m + rcls
                    for c in range(8):
                        kb.mm(ps[:, 0:256], hT[:, c, st: st + dil * 127 + 1: dil], wq[:, c, 512:768],
                              start=(c == 0), stop=(c == 7), r=["hT", "wq"], w=[pk])
                    kb.cp("dve" if ti % 2 else "act", V3[:, p_, ti, :, 0:64], ps[:, 0:256].rearrange("p (h c) -> p h c", c=64), r=[pk], w=["V3"])
            for hl in range(4):
                h = hg * 4 + hl
                fc, pb = hl // 2, 64 * (hl % 2)
                kb.memset("pool", numA[:], 0.0, w=["numA"])
                units = []
                for p_, dil in enumerate(DILS):
                    nbk = 16 // dil
                    for rcls in range(dil):
                        for m in range(nbk):
                            units.append((p_, dil, rcls, m, nbk))
                pend = []
                ui = 0

                def second(u, ui_):
                    p_, dil, rcls, m, nbk = u
                    nq = 256 if m + 1 < nbk else 128
                    ptk = "PT%d" % (ui_ % NL)
                    pt = PT[ui_ % NL]
                    pnk = "psK%d" % (NL + ui_ % 3)
                    pn = psN[ui_ % 3]
                    ti = rcls * nbk + m
                    kb.mm(pn[0:65, 0:nq], V3[:, p_, ti, hl, :], pt[:, 0:nq], r=[ptk, "V3"], w=[pnk])
                    st = dil * 128 * m + rcls
                    sl = slice(st, st + dil * (nq - 1) + 1, dil)
                    kb.tt("dve", numA[:, sl], numA[:, sl], pn[0:65, 0:nq], ALU.add, r=[pnk, "numA"], w=["numA"])

                for u in units:
                    p_, dil, rcls, m, nbk = u
                    nq = 256 if m + 1 < nbk else 128
                    plk = "psK%d" % (ui % NL)
                    pl = psL[ui % NL]
                    ptk = "PT%d" % (ui % NL)
                    pt = PT[ui % NL]
                    st = dil * 128 * m + rcls
                    kb.mm(pl[:, 0:nq], KT[pb:pb + 64, fc, st: st + dil * 127 + 1: dil],
                          QT[pb:pb + 64, fc, st: st + dil * (nq - 1) + 1: dil], start=True, stop=False,
                          r=["KT", "QT"], w=[plk])
                    kb.mm(pl[:, 0:nq], identB[:], biasS[:, p_ * 8 + h, 0:nq], start=False, stop=True,
                          r=["identB", "biasS"], w=[plk])
                    kb.act(pt[:, 0:nq], pl[:, 0:nq], AF.Exp, r=[plk], w=[ptk])
                    pend.append((u, ui))
                    ui += 1
                    if len(pend) > NL - 1:
                        second(*pend.pop(0))
                while pend:
                    second(*pend.pop(0))
                kb.op("dve", lambda e: e.reciprocal(numA[64:65, :], numA[64:65, :]), r=["numA"], w=["numA"])
                for tq in range(4):
                    pb_, pbk_ = pbk[NL + tq % 3], "psK%d" % (NL + tq % 3)
                    kb.mm(pb_[0:64, :], ones128[64:65, 0:64], numA[64:65, tq * 512:(tq + 1) * 512], r=["numA", "ones128"], w=[pbk_])
                    kb.tt("dve", yat[hl % 2][:, tq * 512:(tq + 1) * 512], numA[0:64, tq * 512:(tq + 1) * 512], pb_[0:64, :], ALU.mult,
                          r=["numA", pbk_], w=["yat%d" % (hl % 2)])
                kb.dma(q(), mixD[b, 512 + h * 64:512 + (h + 1) * 64, :], yat[hl % 2][:], r=["yat%d" % (hl % 2)], w=["mixD"])
            kb.phase_end()
        if stop_after == "B":
            bes.close()
            break

        C0 = float(np.exp(-0.5))
        for hp in range(int(os.environ.get('DBG_NHP', '4'))):
            kb.phase_begin()
            Tn = ("r", "k", "v", "a", "kk", "lw", "cum", "x")
            T = {n: kb.sb("T_" + n, [128, S], F32) for n in Tn}
            T_g = kb.sb("T_g", [128, S], BF16)
            T_bon = kb.sb("T_bon", [128, S], BF16)
            Hs = [kb.sb("Hs%d" % i, [64, 17, 64], F32) for i in range(2)]
            psAll = [kb.ps("psC%d" % i, [128, 512]) for i in range(8)]
            cnt = {"b": 0, "s": 0, "e": 0}

            def big():
                i = cnt["b"] % 3
                cnt["b"] += 1
                return psAll[i], "psC%d" % i

            def sml():
                i = cnt["s"] % 8
                cnt["s"] += 1
                return psAll[i], "psC%d" % i

            def ev2():
                cnt["e"] += 1
                return "dve" if cnt["e"] % 2 else "act"

            K_ = lambda n, tq: "T_%s.%d" % (n, tq)
            wres = ExitStack()
            kb.uid += 1
            wr = wres.enter_context(nc.sbuf_tensor("wr_%d" % kb.uid, [128, 8, 6, 128], BF16))
            for j in range(3):
                kb.dma(q(), wr[:, :, j, :], Wa[:, j * 512 + hp * 128: j * 512 + (hp + 1) * 128].rearrange("(c p) f -> p c f", p=128),
                       r=["WaD"], w=["wr"])
                kb.dma(q(), wr[:, :, 3 + j, :], Wb[:, j * 512 + hp * 128: j * 512 + (hp + 1) * 128].rearrange("(c p) f -> p c f", p=128),
                       r=["WbD"], w=["wr"])
            hcol = lambda i: hv[:, i, hp:hp + 1]
            fcols = slice(hp * 128, (hp + 1) * 128)
            TQ = [(tq, slice(tq * 512, (tq + 1) * 512)) for tq in range(4)]
            for tq, ts_ in TQ:
                for j, n in enumerate(("r", "k", "v")):
                    ps, pk = big()
                    for c in range(8):
                        kb.mm(ps[:], wr[:, c, j, :], hT[:, c, 1 + tq * 512:1 + (tq + 1) * 512], start=(c == 0), stop=False,
                              r=["hT", "wr"], w=[pk])
                        kb.mm(ps[:], wr[:, c, 3 + j, :], hT[:, c, tq * 512:(tq + 1) * 512], start=False, stop=(c == 7),
                              r=["hT", "wr"], w=[pk])
                    kb.cp(ev2(), T[n][:, ts_], ps[:], r=[pk], w=[K_(n, tq)])
                ps, pk = big()
                kb.mm(ps[:], W2A2[0:64, fcols], l1wa[0:64, ts_], r=["l1wa", "W2A2"], w=[pk])
                kb.act(T["lw"][:, ts_], ps[:], AF.Sigmoid, bias=hcol(V_W0), r=[pk, "hv"], w=[K_("lw", tq)])
                ps, pk = big()
                kb.mm(ps[:], W2A2[64:128, fcols], l1wa[64:128, ts_], r=["l1wa", "W2A2"], w=[pk])
                kb.act(T["a"][:, ts_], ps[:], AF.Sigmoid, bias=hcol(V_A0), r=[pk, "hv"], w=[K_("a", tq)])
                ps, pk = big()
                kb.mm(ps[:], G2[:, fcols], l1g[:, ts_], r=["l1g", "G2"], w=[pk])
                kb.cp("dve", T_g[:, ts_], ps[:], r=[pk], w=[K_("g", tq)])
            kb.sync_all()
            wres.close()
            for tq, ts_ in TQ:
                kb.ts("dve", T["kk"][:, ts_], T["k"][:, ts_], hcol(V_KK), None, ALU.mult, r=[K_("k", tq), "hv"], w=[K_("kk", tq)])
                kb.tt("dve", T["x"][:, ts_], T["kk"][:, ts_], T["kk"][:, ts_], ALU.mult, r=[K_("kk", tq)], w=[K_("x", tq)])
                ps, pk = big()
                kb.mm(ps[:], BDones[:], T["x"][:, ts_], r=[K_("x", tq), "BD"], w=[pk])
                kb.ts("dve", T["x"][:, ts_], ps[:], 1e-24, None, ALU.max, r=[pk], w=[K_("x", tq)])
            kb.sync_all()
            for tq, ts_ in TQ:
                kb.act(T["x"][:, ts_], T["x"][:, ts_], AF.Sqrt, r=[K_("x", tq)], w=[K_("x", tq)])
            kb.sync_all()
            for tq, ts_ in TQ:
                kb.op("dve", lambda e, ts_=ts_: e.reciprocal(T["x"][:, ts_], T["x"][:, ts_]), r=[K_("x", tq)], w=[K_("x", tq)])
                kb.tt("dve", T["kk"][:, ts_], T["kk"][:, ts_], T["x"][:, ts_], ALU.mult, r=[K_("kk", tq), K_("x", tq)], w=[K_("kk", tq)])
                kb.ts("dve", T["x"][:, ts_], T["a"][:, ts_], hcol(V_KA), hcol(V_1MKA), ALU.mult, ALU.add,
                      r=[K_("a", tq), "hv"], w=[K_("x", tq)])
                kb.tt("dve", T["k"][:, ts_], T["k"][:, ts_], T["x"][:, ts_], ALU.mult, r=[K_("k", tq), K_("x", tq)], w=[K_("k", tq)])
                kb.stt("dve", T["x"][:, ts_], T["r"][:, ts_], hcol(V_RK), T["k"][:, ts_], ALU.mult, ALU.mult,
                       r=[K_("r", tq), K_("k", tq), "hv"], w=[K_("x", tq)])
                ps, pk = big()
                kb.mm(ps[:], BDones[:], T["x"][:, ts_], r=[K_("x", tq), "BD"], w=[pk])
                kb.tt("dve", T_bon[:, ts_], ps[:], T["v"][:, ts_], ALU.mult, r=[pk, K_("v", tq)], w=[K_("bon", tq)])
                for c4 in range(4):
                    cs_ = slice(tq * 512 + c4 * 128, tq * 512 + (c4 + 1) * 128)
                    kb.op("dve", lambda e, cs_=cs_: e.tensor_tensor_scan(T["cum"][:, cs_], ones128[:, 0:128], T["lw"][:, cs_], 0.0, ALU.mult, ALU.add),
                          r=[K_("lw", tq), "ones128"], w=[K_("cum", tq)])
                kb.tt("dve", T["x"][:, ts_], T["cum"][:, ts_], T["lw"][:, ts_], ALU.subtract, r=[K_("cum", tq), K_("lw", tq)], w=[K_("x", tq)])
            kb.sync_all()
            for tq, ts_ in TQ:
                kb.act(T["x"][:, ts_], T["x"][:, ts_], AF.Exp, scale=-C0, r=[K_("x", tq)], w=[K_("x", tq)])
                kb.act(T["lw"][:, ts_], T["cum"][:, ts_], AF.Exp, scale=C0, r=[K_("cum", tq)], w=[K_("lw", tq)])
                kb.act(T["cum"][:, ts_], T["cum"][:, ts_], AF.Exp, scale=-C0, r=[K_("cum", tq)], w=[K_("cum", tq)])
            kb.sync_all()
            for tq, ts_ in TQ:
                kb.stt("dve", T["x"][:, ts_], T["kk"][:, ts_], -1.0, T["x"][:, ts_], ALU.mult, ALU.mult,
                       r=[K_("kk", tq), K_("x", tq)], w=[K_("x", tq)])
                kb.tt("dve", T["kk"][:, ts_], T["kk"][:, ts_], T["a"][:, ts_], ALU.mult, r=[K_("kk", tq), K_("a", tq)], w=[K_("kk", tq)])
                kb.tt("dve", T["kk"][:, ts_], T["kk"][:, ts_], T["lw"][:, ts_], ALU.mult, r=[K_("kk", tq), K_("lw", tq)], w=[K_("kk", tq)])
                kb.tt("dve", T["k"][:, ts_], T["k"][:, ts_], T["lw"][:, ts_], ALU.mult, r=[K_("k", tq), K_("lw", tq)], w=[K_("k", tq)])
                kb.tt("dve", T["r"][:, ts_], T["r"][:, ts_], T["cum"][:, ts_], ALU.mult, r=[K_("r", tq), K_("cum", tq)], w=[K_("r", tq)])
            kb.sync_all()
            At, Bt, Kt, Rt, Gam, Vt = T["x"], T["kk"], T["k"], T["r"], T["cum"], T["v"]
            YT = [T["lw"], T["a"]]
            ytn = ["lw", "a"]
            KW = int(os.environ.get("SCAN_WAYS", "6"))
            PPs = [[kb.sb("PP%d_%d" % (sl, i), [128, 256], BF16) for i in range(7)] for sl in range(KW)]
            XBs = [[kb.sb("XB%d_%d" % (sl, i), [128, 128], BF16) for i in range(2)] for sl in range(KW)]
            NAs = [kb.sb("NA%d" % sl, [128, 256], F32) for sl in range(KW)]
            ARs = [kb.sb("AR%d" % sl, [128, 256], F32) for sl in range(KW)]
            XXs = [[kb.sb("XX%d_%d" % (sl, i), [128, 128], F32) for i in range(4)] for sl in range(KW)]
            tokms = [kb.sb("tokm%d" % sl, [128, 4, 64], F32) for sl in range(KW)]
            blkls = [kb.sb("blkl%d" % sl, [128, 2, 128], F32) for sl in range(KW)]
            RhTs = [kb.sb("RhT%d" % sl, [64, 128], F32) for sl in range(KW)]
            YiTs = [kb.sb("YiT%d" % sl, [64, 128], F32) for sl in range(KW)]
            Gts = [kb.sb("Gt%d" % sl, [64, 64], F32) for sl in range(KW)]
            Fpps = [kb.sb("Fpp%d" % sl, [64, 64], F32) for sl in range(KW)]
            for e_ in range(2):
                kb.memset("pool", Hs[e_][:, 0, :], 0.0, w=["Hs%d_0" % e_])

            def unit(e_, c, sl):
                pb = 64 * e_
                pbs = slice(pb, pb + 64)
                idp = identF[pbs, pb:pb + 64]
                PP, NA, AR, XX, tokm, blkl = PPs[sl], NAs[sl], ARs[sl], XXs[sl], tokms[sl], blkls[sl]
                XB = XBs[sl]
                RhT, YiT, Gt, Fpp = RhTs[sl], YiTs[sl], Gts[sl], Fpps[sl]
                P2 = "_%d" % sl
                tq = c // 4
                cs_ = slice(c * 128, (c + 1) * 128)
                gl = Gam[pbs, c * 128 + 127:c * 128 + 128]
                kb.ts("dve", blkl[pbs, 0, :], Bt[pbs, cs_], gl, None, ALU.mult, r=[K_("kk", tq), K_("cum", tq)], w=["blkl" + P2])
                kb.act(blkl[pbs, 1, :], Kt[pbs, cs_], AF.Copy, scale=gl, r=[K_("k", tq), K_("cum", tq)], w=["blkl" + P2])
                ps, pk = sml()
                kb.tr(ps[:, 0:64], At[pbs, cs_], idp, r=[K_("x", tq), "identF"], w=[pk])
                kb.tr(ps[:, 64:128], blkl[pbs, 0, :], idp, r=["blkl" + P2, "identF"], w=[pk])
                kb.tr(ps[:, 128:192], blkl[pbs, 1, :], idp, r=["blkl" + P2, "identF"], w=[pk])
                kb.tr(ps[:, 192:256], Vt[pbs, cs_], idp, r=[K_("v", tq), "identF"], w=[pk])
                kb.cp("act", tokm[:].rearrange("p a b -> p (a b)"), ps[:, 0:256], r=[pk], w=["tokm" + P2])
                yield
                ps, pk = sml()
                kb.mm(ps[:, 0:128], Bt[pbs, cs_], At[pbs, cs_], r=[K_("kk", tq), K_("x", tq)], w=[pk])
                kb.mm(ps[:, 128:256], Kt[pbs, cs_], At[pbs, cs_], r=[K_("k", tq), K_("x", tq)], w=[pk])
                kb.mm(ps[:, 256:384], At[pbs, cs_], Bt[pbs, cs_], r=[K_("kk", tq), K_("x", tq)], w=[pk])
                kb.tt("dve", NA[:], ps[:, 0:256], m2SU[:], ALU.mult, r=[pk, "m2SU"], w=["NA" + P2])
                kb.tt("dve", PP[0][:, 0:128], ps[:, 256:384], mSL[:], ALU.mult, r=[pk, "mSL"], w=["PP0n" + P2])
                kb.cp("act", PP[0][:, 128:256], NA[:, 0:128], r=["NA" + P2], w=["PP0t" + P2])
                ps, pk = sml()
                kb.mm(ps[:, 0:128], Bt[pbs, cs_], Rt[pbs, cs_], r=[K_("kk", tq), K_("r", tq)], w=[pk])
                kb.mm(ps[:, 128:256], Kt[pbs, cs_], Rt[pbs, cs_], r=[K_("k", tq), K_("r", tq)], w=[pk])
                kb.tt("dve", AR[:], ps[:, 0:256], m2IU[:], ALU.mult, r=[pk, "m2IU"], w=["AR" + P2])
                yield
                xi = 0
                ps, pk = sml()
                kb.mm(ps[:, 0:64], NA[:, 128:256], tokm[:, 3, :], r=["NA" + P2, "tokm" + P2], w=[pk])
                kb.cp("act", XX[0][:, 0:64], tokm[:, 0, :], r=["tokm" + P2], w=["XX0" + P2])
                kb.cp("act", XX[0][:, 64:128], ps[:, 0:64], r=[pk], w=["XX0" + P2])
                kb.cp("act", XB[0][:], XX[0][:], r=["XX0" + P2], w=["XB0" + P2])
                yield
                for i in range(7):
                    ps, pk = sml()
                    kb.mm(ps[:, 0:128], PP[i][:, 128:256], XB[i % 2][:], r=["PP%dt" % i + P2, "XB%d" % (i % 2) + P2], w=[pk])
                    if i < 6:
                        ps2, pk2 = sml()
                        if i < 5:
                            kb.mm(ps2[:, 0:128], PP[i][:, 128:256], PP[i][:, 0:128], r=["PP%dt" % i + P2, "PP%dn" % i + P2], w=[pk2])
                        kb.mm(ps2[:, 128:256], PP[i][:, 0:128], PP[i][:, 128:256], r=["PP%dt" % i + P2, "PP%dn" % i + P2], w=[pk2])
                        if i < 5:
                            kb.cp("act", PP[i + 1][:], ps2[:, 0:256], r=[pk2], w=["PP%dn" % (i + 1) + P2, "PP%dt" % (i + 1) + P2])
                        else:
                            kb.cp("act", PP[i + 1][:, 128:256], ps2[:, 128:256], r=[pk2], w=["PP%dt" % (i + 1) + P2])
                    nx = (xi + 1) % 4
                    kb.tt("dve", XX[nx][:], ps[:, 0:128], XX[xi][:], ALU.add, r=[pk, "XX%d" % xi + P2], w=["XX%d" % nx + P2])
                    if i < 6:
                        kb.cp("act", XB[(i + 1) % 2][:], XX[nx][:], r=["XX%d" % nx + P2], w=["XB%d" % ((i + 1) % 2) + P2])
                    xi = nx
                    yield
                Xf = XX[xi]
                xk = "XX%d" % xi + P2
                ps, pk = sml()
                kb.mm(ps[0:64, 0:128], Xf[:, 0:64], AR[:, 0:128], r=[xk, "AR" + P2], w=[pk])
                kb.mm(ps[0:64, 256:320], Xf[:, 0:64], tokm[:, 1, :], r=[xk, "tokm" + P2], w=[pk])
                ps2, pk2 = sml()
                kb.mm(ps2[0:64, 128:256], Xf[:, 64:128], AR[:, 0:128], start=True, stop=False, r=[xk, "AR" + P2], w=[pk2])
                kb.mm(ps2[0:64, 128:256], tokm[:, 3, :], AR[:, 128:256], start=False, stop=True, r=["tokm" + P2, "AR" + P2], w=[pk2])
                kb.mm(ps2[0:64, 320:384], tokm[:, 1, :], Xf[:, 64:128], start=True, stop=False, r=[xk, "tokm" + P2], w=[pk2])
                kb.mm(ps2[0:64, 320:384], tokm[:, 2, :], tokm[:, 3, :], start=False, stop=True, r=["tokm" + P2], w=[pk2])
                kb.tt("dve", RhT[:], ps[0:64, 0:128], Rt[pbs, cs_], ALU.add, r=[pk, K_("r", tq)], w=["RhT" + P2])
                kb.stt("dve", Gt[:], idp, gl, ps[0:64, 256:320], ALU.mult, ALU.add, r=[pk, "identF", K_("cum", tq)], w=["Gt" + P2])
                kb.cp("act", YiT[:], ps2[0:64, 128:256], r=[pk2], w=["YiT" + P2])
                kb.cp("act", Fpp[:], ps2[0:64, 320:384], r=[pk2], w=["Fpp" + P2])
                yield
                ps, pk = sml()
                HK = "Hs%d_%d" % (e_, c)
                kb.mm(ps[0:64, 0:128], Hs[e_][:, c, :], RhT[:], r=[HK, "RhT" + P2], w=[pk])
                kb.mm(ps[0:64, 128:192], Gt[:], Hs[e_][:, c, :], r=[HK, "Gt" + P2], w=[pk])
                kb.tt("dve", Hs[e_][:, c + 1, :], ps[0:64, 128:192], Fpp[:], ALU.add, r=[pk, "Fpp" + P2], w=["Hs%d_%d" % (e_, c + 1)])
                kb.tt("dve", YT[e_][0:64, cs_], ps[0:64, 0:128], YiT[:], ALU.add, r=[pk, "YiT" + P2], w=[K_(ytn[e_], tq)])
                yield

            todo = [(e_, c) for c in range(int(os.environ.get('DBG_NC', '16'))) for e_ in range(2)]
            active = [None] * KW
            while todo or any(g is not None for g in active):
                started = False
                for sl in range(KW):
                    if active[sl] is None and todo and not started:
                        e_, c = todo.pop(0)
                        active[sl] = unit(e_, c, sl)
                        started = True
                    if active[sl] is not None:
                        try:
                            next(active[sl])
                        except StopIteration:
                            active[sl] = None
            kb.sync_all()
            for tq, ts_ in TQ:
                ps, pk = big()
                kb.mm(ps[:], csm[:, 0:128], YT[0][0:64, ts_], start=True, stop=False, r=[K_("lw", tq), "csm"], w=[pk])
                kb.mm(ps[:], csm[:, 128:256], YT[1][0:64, ts_], start=False, stop=True, r=[K_("a", tq), "csm"], w=[pk])
                kb.cp("dve", T["x"][:, ts_], ps[:], r=[pk], w=[K_("x", tq)])
                kb.tt("dve", T["kk"][:, ts_], T["x"][:, ts_], T["x"][:, ts_], ALU.mult, r=[K_("x", tq)], w=[K_("kk", tq)])
                ps, pk = big()
                kb.mm(ps[:], BDmean[:], T["kk"][:, ts_], r=[K_("kk", tq), "BD"], w=[pk])
                kb.ts("dve", T["kk"][:, ts_], ps[:], LNX_EPS, None, ALU.add, r=[pk], w=[K_("kk", tq)])
            kb.sync_all()
            for tq, ts_ in TQ:
                kb.act(T["kk"][:, ts_], T["kk"][:, ts_], AF.Sqrt, r=[K_("kk", tq)], w=[K_("kk", tq)])
            kb.sync_all()
            for tq, ts_ in TQ:
                kb.op("dve", lambda e, ts_=ts_: e.reciprocal(T["kk"][:, ts_], T["kk"][:, ts_]), r=[K_("kk", tq)], w=[K_("kk", tq)])
                kb.tt("dve", T["x"][:, ts_], T["x"][:, ts_], T["kk"][:, ts_], ALU.mult, r=[K_("x", tq), K_("kk", tq)], w=[K_("x", tq)])
                kb.ts("dve", T["x"][:, ts_], T["x"][:, ts_], hcol(V_LG), hcol(V_LB), ALU.mult, ALU.add, r=[K_("x", tq), "hv"], w=[K_("x", tq)])
                kb.tt("dve", T["x"][:, ts_], T["x"][:, ts_], T_bon[:, ts_], ALU.add, r=[K_("x", tq), K_("bon", tq)], w=[K_("x", tq)])
                kb.tt("dve", T_bon[:, ts_], T["x"][:, ts_], T_g[:, ts_], ALU.mult, r=[K_("x", tq), K_("g", tq), K_("bon", tq)], w=[K_("bon", tq)])
                kb.dma(q(), mixD[b, hp * 128:(hp + 1) * 128, ts_], T_bon[:, ts_], r=[K_("bon", tq)], w=["mixD"])
            kb.phase_end()
        if stop_after == "C":
            bes.close()
            break
        kb.phase_begin()
        Wo = kb.sb("Wo", [128, 8, D], BF16)
        mixT = kb.sb("mixT", [128, 8, S], BF16)
        kb.dma("sp", mixT[:, 0:4, :], mixD[b, 0:512, :].rearrange("(c p) t -> p c t", p=128), r=["mixD"], w=["mixT"])
        kb.dma("act", mixT[:, 4:8, :], mixD[b, 512:1024, :].rearrange("(c p) t -> p c t", p=128), r=["mixD"], w=["mixT"])
        Wr = kb.sb("Wr", [128, 8, 36], F32)
        rbb = kb.sb("rbb", [128, 36], F32)
        g1b = kb.sb("g1b", [128, D], F32)
        b1b = kb.sb("b1b", [128, D], F32)
        xt = [kb.sb("xtD%d" % i, [128, D], F32) for i in range(4)]
        yt = [kb.sb("ytD%d" % i, [128, D], F32) for i in range(4)]
        sqt2 = [kb.sb("sqD%d" % i, [128, D], F32) for i in range(4)]
        x1Tb = [kb.sb("x1Tb%d" % i, [128, 8, 128], BF16) for i in range(4)]
        x1Tf = [kb.sb("x1Tf%d" % i, [128, 8, 128], F32) for i in range(4)]
        st = [kb.sb("stD%d" % i, [128, 8], F32) for i in range(4)]
        L16 = kb.sb("L16", [128, 16, 36], F32)
        m16 = kb.sb("m16", [128, 16], F32)
        wg16 = kb.sb("wg16", [128, 16], F32)
        m116 = kb.sb("m116", [128, 16], F32)
        m216 = kb.sb("m216", [128, 16], F32)
        e416 = kb.sb("e416", [128, 16, 4], F32)
        gm16 = kb.sb("gm16", [128, 16, 4], F32)
        lem16 = kb.sb("lem16", [128, 16, 32], F32)
        lem216 = kb.sb("lem216", [128, 16, 32], F32)
        mk116 = kb.sb("mk116", [128, 16, 32], F32)
        psD = [kb.ps("psD%d" % i, [128, 512]) for i in range(4)]
        psT = [kb.ps("psT%d" % i, [128, 512]) for i in range(3)]
        psR = kb.ps("psR", [128, 512])
        kb.dma("pool", Wo[:], w_o.rearrange("(c p) f -> p c f", p=128), w=["Wo"])
        kb.dma("sp", Wr[:], router.rearrange("(c p) f -> p c f", p=128), w=["Wr"])
        kb.dma("sp", rbb[:], router_b.partition_broadcast(128), w=["rbb"])
        kb.dma("sp", g1b[:], ln1_g.partition_broadcast(128), w=["g1b"])
        kb.dma("act", b1b[:], ln1_b.partition_broadcast(128), w=["b1b"])
        BIG = 1.0e4
        def tileD(t):
            i = t % 4
            I2 = "%d" % i
            tsl = slice(t * 128, (t + 1) * 128)
            kb.dma(q(), xt[i][:], x[b, tsl, :], w=["xtD" + I2])
            for dh in range(2):
                ps, pk = psD[(2 * t + dh) % 4], "psD%d" % ((2 * t + dh) % 4)
                for c in range(8):
                    kb.mm(ps[:], mixT[:, c, tsl], Wo[:, c, dh * 512:(dh + 1) * 512], start=(c == 0), stop=(c == 7),
                          r=["mixT", "Wo"], w=[pk])
                kb.stt("dve", yt[i][:, dh * 512:(dh + 1) * 512], xt[i][:, dh * 512:(dh + 1) * 512], ALPHA, ps[:], ALU.mult, ALU.add,
                       r=[pk, "xtD" + I2], w=["ytD" + I2])
            s_ = st[i]
            kb.op("dve", lambda e, i=i, s_=s_: e.reduce_sum(s_[:, 0:1], yt[i][:], AX.X), r=["ytD" + I2], w=["stD" + I2])
            yield
            kb.ts("dve", s_[:, 1:2], s_[:, 0:1], -1.0 / D, None, ALU.mult, r=["stD" + I2], w=["stD" + I2])
            kb.ts("dve", yt[i][:], yt[i][:], s_[:, 1:2], None, ALU.add, r=["ytD" + I2, "stD" + I2], w=["ytD" + I2])
            kb.tt("pool", sqt2[i][:], yt[i][:], yt[i][:], ALU.mult, r=["ytD" + I2], w=["sqD" + I2])
            yield
            kb.op("dve", lambda e, s_=s_, q_=sqt2[i]: e.reduce_sum(s_[:, 2:3], q_[:], AX.X), r=["sqD" + I2], w=["stD" + I2])
            kb.ts("dve", s_[:, 3:4], s_[:, 2:3], 1.0 / D, LN_EPS, ALU.mult, ALU.add, r=["stD" + I2], w=["stD" + I2])
            kb.act(s_[:, 4:5], s_[:, 3:4], AF.Sqrt, r=["stD" + I2], w=["stD" + I2])
            yield
            kb.op("dve", lambda e, s_=s_: e.reciprocal(s_[:, 5:6], s_[:, 4:5]), r=["stD" + I2], w=["stD" + I2])
            kb.stt("dve", yt[i][:], yt[i][:], s_[:, 5:6], g1b[:], ALU.mult, ALU.mult, r=["ytD" + I2, "stD" + I2, "g1b"], w=["ytD" + I2])
            kb.tt("pool", yt[i][:], yt[i][:], b1b[:], ALU.add, r=["ytD" + I2, "b1b"], w=["ytD" + I2])
            yield
            kb.dma(q(), x1D[b, tsl, :], yt[i][:], r=["ytD" + I2], w=["x1D"])
            if dbg and dbg[0] == "x1" and b == 0:
                kb.dma("sp", dbg_out[tsl, :], yt[i][:], r=["ytD" + I2], w=["dbgout"])
            for half in range(2):
                ps, pk = psT[(2 * t + half) % 3], "psT%d" % ((2 * t + half) % 3)
                for cc in range(4):
                    c = half * 4 + cc
                    kb.tr(ps[:, cc * 128:(cc + 1) * 128], yt[i][:, c * 128:(c + 1) * 128], identF[:], r=["ytD" + I2, "identF"], w=[pk])
                kb.cp("act", x1Tb[i][:, half * 4:half * 4 + 4, :], ps[:].rearrange("p (c t) -> p c t", t=128), r=[pk], w=["x1Tb" + I2])
                kb.cp("dve", x1Tf[i][:, half * 4:half * 4 + 4, :], ps[:].rearrange("p (c t) -> p c t", t=128), r=[pk], w=["x1Tf" + I2])
            kb.dma(q(), x1TD[b].rearrange("(c p) t -> p c t", p=128)[:, :, tsl], x1Tb[i][:], r=["x1Tb" + I2], w=["x1TD"])
            yield
            for c in range(8):
                kb.mm(psR[:, 0:36], x1Tf[i][:, c, :], Wr[:, c, :], start=(c == 0), stop=(c == 7), r=["x1Tf" + I2, "Wr"], w=["psR"])
            kb.tt("dve", L16[:, t, :], psR[:, 0:36], rbb[:], ALU.add, r=["psR", "rbb"], w=["L16"])

        def run_rr(gens, ways):
            active = [None] * ways
            gens = list(gens)
            while gens or any(g is not None for g in active):
                started = False
                for sl in range(ways):
                    if active[sl] is None and gens and not started:
                        active[sl] = gens.pop(0)
                        started = True
                    if active[sl] is not None:
                        try:
                            next(active[sl])
                        except StopIteration:
                            active[sl] = None

        run_rr([tileD(t) for t in range(16)], 4)
        RK = "rt16"
        bc = lambda ap, n: ap.unsqueeze(2).to_broadcast([128, 16, n])
        lg16 = L16[:, :, 0:4]
        kb.op("dve", lambda e: e.reduce_max(m16[:], lg16, AX.X), r=["L16"], w=[RK])
        kb.tt("dve", e416[:], lg16, bc(m16[:], 4), ALU.subtract, r=["L16", RK], w=[RK])
        kb.act(e416[:], e416[:], AF.Exp, r=[RK], w=[RK])
        kb.op("dve", lambda e: e.reduce_sum(wg16[:], e416[:], AX.X), r=[RK], w=[RK])
        kb.op("dve", lambda e: e.reciprocal(wg16[:], wg16[:]), r=[RK], w=[RK])
        kb.tt("dve", gm16[:], lg16, bc(m16[:], 4), ALU.is_equal, r=["L16", RK], w=[RK])
        kb.ts("dve", gm16[:], gm16[:], BIG, -BIG, ALU.mult, ALU.add, r=[RK], w=[RK])
        for g_ in range(4):
            kb.tt("dve", lem16[:, :, g_ * 8:(g_ + 1) * 8], L16[:, :, 4 + g_ * 8:12 + g_ * 8], bc(gm16[:, :, g_], 8), ALU.add,
                  r=["L16", RK], w=[RK])
        kb.op("dve", lambda e: e.reduce_max(m116[:], lem16[:], AX.X), r=[RK], w=[RK])
        kb.tt("dve", mk116[:], lem16[:], bc(m116[:], 32), ALU.is_equal, r=[RK], w=[RK])
        kb.stt("dve", lem216[:], mk116[:], -BIG, lem16[:], ALU.mult, ALU.add, r=[RK], w=[RK])
        kb.op("dve", lambda e: e.reduce_max(m216[:], lem216[:], AX.X), r=[RK], w=[RK])
        kb.tt("dve", lem16[:], lem216[:], bc(m216[:], 32), ALU.is_equal, r=[RK], w=[RK])
        kb.tt("dve", m216[:], m216[:], m116[:], ALU.subtract, r=[RK], w=[RK])
        kb.act(m216[:], m216[:], AF.Exp, r=[RK], w=[RK])
        kb.ts("dve", m116[:], m216[:], 1.0, None, ALU.add, r=[RK], w=[RK])
        kb.op("dve", lambda e: e.reciprocal(m116[:], m116[:]), r=[RK], w=[RK])
        kb.tt("dve", m216[:], m216[:], m116[:], ALU.mult, r=[RK], w=[RK])
        kb.tt("dve", m116[:], m116[:], wg16[:], ALU.mult, r=[RK], w=[RK])
        kb.tt("dve", m216[:], m216[:], wg16[:], ALU.mult, r=[RK], w=[RK])
        kb.tt("dve", mk116[:], mk116[:], bc(m116[:], 32), ALU.mult, r=[RK], w=[RK])
        kb.tt("dve", lem16[:], lem16[:], bc(m216[:], 32), ALU.mult, r=[RK], w=[RK])
        kb.tt("dve", mk116[:], mk116[:], lem16[:], ALU.add, r=[RK], w=[RK])
        kb.dma("sp", gateD[b].rearrange("(t p) e -> p t e", p=128), mk116[:], r=[RK], w=["gateD"])
        kb.phase_end()
        bes.close()
        bes = ExitStack()
        if stop_after == "D":
            break

        acc = bes.enter_context(nc.sbuf_tensor("acc_b%d" % b, [128, 16, D], F32))
        x1T = bes.enter_context(nc.sbuf_tensor("x1T_b%d" % b, [128, 8, S], BF16))
        gate = bes.enter_context(nc.sbuf_tensor("gate_b%d" % b, [128, 16, 32], F32))
        kb.phase_begin()
        wgu = [kb.sb("wgu%d" % i, [128, 8, 1024], BF16) for i in range(2)]
        wdn = [kb.sb("wdn%d" % i, [128, 4, D], BF16) for i in range(2)]
        hTe = kb.sb("hTe", [128, 4, S], BF16)
        sgt = [kb.sb("sgt%d" % i, [128, 512], BF16) for i in range(2)]
        psG = [kb.ps("psG%d" % i, [128, 512]) for i in range(2)]
        psU = [kb.ps("psU%d" % i, [128, 512]) for i in range(2)]
        psY = [kb.ps("psY%d" % i, [128, 512]) for i in range(4)]
        kb.dma("sp", x1T[:], x1TD[b].rearrange("(c p) t -> p c t", p=128), r=["x1TD"], w=["x1T"])
        kb.dma("act", gate[:], gateD[b].rearrange("(t p) e -> p t e", p=128), r=["gateD"], w=["gate"])
        for t in range(16):
            kb.dma(q(), acc[:, t, :], x1D[b, t * 128:(t + 1) * 128, :], r=["x1D"], w=["acc%d" % t])
            kb.ts("pool", acc[:, t, :], acc[:, t, :], ALPHA, None, ALU.mult, r=["acc%d" % t], w=["acc%d" % t])
        NEXP = int(os.environ.get("DBG_NEXP", "32"))
        ui = 0
        for e_ in range(NEXP):
            i = e_ % 2
            I2 = "%d" % i
            kb.dma("pool", wgu[i][:, :, 0:512], w_gate[e_].rearrange("(c p) f -> p c f", p=128), w=["wgu" + I2])
            kb.dma("pool", wgu[i][:, :, 512:1024], w_up[e_].rearrange("(c p) f -> p c f", p=128), w=["wgu" + I2])
            kb.dma("pool", wdn[i][:], w_down[e_].rearrange("(c p) f -> p c f", p=128), w=["wdn" + I2])
            for tq in range(4):
                for fc in range(4):
                    j = ui % 2
                    ui += 1
                    J2 = "%d" % j
                    for c in range(8):
                        kb.mm(psG[j][:], wgu[i][:, c, fc * 128:(fc + 1) * 128], x1T[:, c, tq * 512:(tq + 1) * 512],
                              start=(c == 0), stop=(c == 7), r=["wgu" + I2, "x1T"], w=["psG" + J2])
                    for c in range(8):
                        kb.mm(psU[j][:], wgu[i][:, c, 512 + fc * 128:512 + (fc + 1) * 128], x1T[:, c, tq * 512:(tq + 1) * 512],
                              start=(c == 0), stop=(c == 7), r=["wgu" + I2, "x1T"], w=["psU" + J2])
                    kb.act(sgt[j][:], psG[j][:], AF.Silu, r=["psG" + J2], w=["sgt" + J2])
                    kb.tt("dve", hTe[:, fc, tq * 512:(tq + 1) * 512], sgt[j][:], psU[j][:], ALU.mult,
                          r=["sgt" + J2, "psU" + J2], w=["hTe%d" % tq])
            for t in range(16):
                for dh in range(2):
                    k = (2 * t + dh) % 4
                    for fc in range(4):
                        kb.mm(psY[k][:], hTe[:, fc, t * 128:(t + 1) * 128], wdn[i][:, fc, dh * 512:(dh + 1) * 512],
                              start=(fc == 0), stop=(fc == 3), r=["hTe%d" % (t // 4), "wdn" + I2], w=["psY%d" % k])
                    kb.stt("dve", acc[:, t, dh * 512:(dh + 1) * 512], psY[k][:], gate[:, t, e_:e_ + 1], acc[:, t, dh * 512:(dh + 1) * 512],
                           ALU.mult, ALU.add, r=["psY%d" % k, "gate", "acc%d" % t], w=["acc%d" % t])
        if dbg and dbg[0] == "accmoe" and b == 0:
            for t in range(16):
                kb.dma("sp", dbg_out[t * 128:(t + 1) * 128, :], acc[:, t, :], r=["acc%d" % t], w=["dbgout"])
        kb.phase_end()
        if stop_after == "E1":
            bes.close()
            break
        kb.phase_begin()
        Gw = kb.sb("Gw", [128, 8, D], BF16)
        Pw = kb.sb("Pw", [128, 2, D], BF16)
        g2b = kb.sb("g2b", [128, D], F32)
        b2b = kb.sb("b2b", [128, D], F32)
        pt_ = [kb.sb("ptE%d" % i, [128, 256], F32) for i in range(4)]
        pTt = [kb.sb("pTt%d" % i, [128, 2, 128], BF16) for i in range(4)]
        sig = [kb.sb("sigE%d" % i, [128, 512], F32) for i in range(2)]
        sqt3 = [kb.sb("sqE%d" % i, [128, D], F32) for i in range(4)]
        st = [kb.sb("stE%d" % i, [128, 8], F32) for i in range(4)]
        psA_ = [kb.ps("psEa%d" % i, [128, 512]) for i in range(2)]
        psB_ = [kb.ps("psEb%d" % i, [128, 512]) for i in range(2)]
        psC_ = [kb.ps("psEc%d" % i, [128, 512]) for i in range(2)]
        kb.dma("pool", Gw[:], ple_gate.rearrange("(c p) f -> p c f", p=128), w=["Gw"])
        kb.dma("pool", Pw[:], ple_proj.rearrange("(c p) f -> p c f", p=128), w=["Pw"])
        kb.dma("sp", g2b[:], ln2_g.partition_broadcast(128), w=["g2b"])
        kb.dma("act", b2b[:], ln2_b.partition_broadcast(128), w=["b2b"])
        def tileE(t):
            i = t % 4
            I2 = "%d" % i
            tsl = slice(t * 128, (t + 1) * 128)
            kb.dma(q(), pt_[i][:], pin[b, tsl, :], w=["ptE" + I2])
            for cc in range(2):
                kb.tr(psC_[i % 2][:, cc * 128:(cc + 1) * 128], pt_[i][:, cc * 128:(cc + 1) * 128], identF[:], r=["ptE" + I2, "identF"], w=["psEc%d" % (i % 2)])
            kb.cp("act", pTt[i][:], psC_[i % 2][:, 0:256].rearrange("p (c t) -> p c t", t=128), r=["psEc%d" % (i % 2)], w=["pTt" + I2])
            yield
            for dh in range(2):
                k = (2 * t + dh) % 2
                K2 = "%d" % k
                dsl = slice(dh * 512, (dh + 1) * 512)
                for c in range(8):
                    kb.mm(psA_[k][:], x1T[:, c, tsl], Gw[:, c, dsl], start=(c == 0), stop=(c == 7), r=["x1T", "Gw"], w=["psEa" + K2])
                kb.act(sig[k][:], psA_[k][:], AF.Sigmoid, r=["psEa" + K2], w=["sigE" + K2])
                for c in range(2):
                    kb.mm(psB_[k][:], pTt[i][:, c, :], Pw[:, c, dsl], start=(c == 0), stop=(c == 1), r=["pTt" + I2, "Pw"], w=["psEb" + K2])
                kb.tt("dve", sig[k][:], sig[k][:], psB_[k][:], ALU.mult, r=["sigE" + K2, "psEb" + K2], w=["sigE" + K2])
                kb.tt("dve", acc[:, t, dsl], acc[:, t, dsl], sig[k][:], ALU.add, r=["sigE" + K2, "acc%d" % t], w=["acc%d" % t])
            y_ = acc[:, t, :]
            AK = "acc%d" % t
            s_ = st[i]
            kb.op("dve", lambda e, y_=y_, s_=s_: e.reduce_sum(s_[:, 0:1], y_, AX.X), r=[AK], w=["stE" + I2])
            yield
            kb.ts("dve", s_[:, 1:2], s_[:, 0:1], -1.0 / D, None, ALU.mult, r=["stE" + I2], w=["stE" + I2])
            kb.ts("dve", y_, y_, s_[:, 1:2], None, ALU.add, r=[AK, "stE" + I2], w=[AK])
            kb.tt("pool", sqt3[i][:], y_, y_, ALU.mult, r=[AK], w=["sqE" + I2])
            yield
            kb.op("dve", lambda e, s_=s_, q_=sqt3[i]: e.reduce_sum(s_[:, 2:3], q_[:], AX.X), r=["sqE" + I2], w=["stE" + I2])
            kb.ts("dve", s_[:, 3:4], s_[:, 2:3], 1.0 / D, LN_EPS, ALU.mult, ALU.add, r=["stE" + I2], w=["stE" + I2])
            kb.act(s_[:, 4:5], s_[:, 3:4], AF.Sqrt, r=["stE" + I2], w=["stE" + I2])
            yield
            kb.op("dve", lambda e, s_=s_: e.reciprocal(s_[:, 5:6], s_[:, 4:5]), r=["stE" + I2], w=["stE" + I2])
            kb.stt("dve", y_, y_, s_[:, 5:6], g2b[:], ALU.mult, ALU.mult, r=[AK, "stE" + I2, "g2b"], w=[AK])
            kb.tt("pool", y_, y_, b2b[:], ALU.add, r=[AK, "b2b"], w=[AK])
            kb.dma(q(), out[b, tsl, :], y_, r=[AK], w=["outD"])
        run_rr([tileE(t) for t in range(16)], 3)
        kb.phase_end()
        bes.close()
    kb.close()
    return nc


def host_inputs(inputs):
    f = lambda a: np.ascontiguousarray(np.asarray(a, dtype=np.float32))
    rel_bias = f(inputs["rel_bias"])
    ext = np.concatenate([rel_bias, np.full((1, 8), NEG, np.float32)], 0)
    idx = bias_index_tables()
    biasT = np.ascontiguousarray(np.transpose(ext[idx], (0, 3, 1, 2)))
    shared = {
        "w_in": f(inputs["w_in"][0]), "mu_rkv": f(inputs["mu_rkv"][0]).reshape(-1), "mu_lora": f(inputs["mu_lora"][0]),
        "w0": f(inputs["w0"][0]), "a0": f(inputs["a0"][0]),
        "w_lora1": f(inputs["w_lora1"][0]), "w_lora2": f(inputs["w_lora2"][0]),
        "a_lora1": f(inputs["a_lora1"][0]), "a_lora2": f(inputs["a_lora2"][0]),
        "g_lora1": f(inputs["g_lora1"][0]), "g_lora2": f(inputs["g_lora2"][0]),
        "k_k": f(inputs["k_k"][0]), "k_a": f(inputs["k_a"][0]), "r_k": f(inputs["r_k"][0]).reshape(-1),
        "lnx_g": f(inputs["lnx_g"][0]), "lnx_b": f(inputs["lnx_b"][0]),
        "biasT": biasT, "w_o": f(inputs["w_o"][0]), "ln1_g": f(inputs["ln1_g"][0]), "ln1_b": f(inputs["ln1_b"][0]),
        "router": np.ascontiguousarray(np.concatenate([f(inputs["router_g"][0]), f(inputs["router_e"][0])], 1)),
        "router_b": np.ascontiguousarray(np.concatenate([f(inputs["router_g_b"][0]), f(inputs["router_e_b"][0])], 0)),
        "w_gate": f(inputs["w_gate"][0]), "w_up": f(inputs["w_up"][0]), "w_down": f(inputs["w_down"][0]),
        "ple_gate": f(inputs["ple_gate"][0]), "ple_proj": f(inputs["ple_proj"][0]),
        "ln2_g": f(inputs["ln2_g"][0]), "ln2_b": f(inputs["ln2_b"][0]),
    }
    return shared


def kernel(**inputs):
    shared = host_inputs(inputs)
    x = np.asarray(inputs["x"], dtype=np.float32)
    p = np.asarray(inputs["p"], dtype=np.float32)[0]
    nc = build_program()
    in_maps = []
    for c in range(8):
        m = dict(shared)
        m["x"] = np.ascontiguousarray(x[c * NB:(c + 1) * NB])
        m["p"] = np.ascontiguousarray(p[c * NB:(c + 1) * NB])
        in_maps.append(m)
    res = run_bass_kernel_spmd(nc, in_maps, core_ids=list(range(8)))
    return np.concatenate([r["out"] for r in res.results], axis=0).astype(np.float32)
```

```python
import numpy as np
import os
from contextlib import ExitStack
import concourse.bass as bass
import concourse.mybir as mybir
from concourse.bass_utils import run_bass_kernel_spmd

F32 = mybir.dt.float32
BF16 = mybir.dt.bfloat16
AF = mybir.ActivationFunctionType
ALU = mybir.AluOpType
AX = mybir.AxisListType

ENGS = ("pe", "dve", "act", "pool", "sp")
NB = 2
S = 2048
D = 1024
NEG = -30000.0
ALPHA = 2.0 ** 0.25
LN_EPS = 1e-5
LNX_EPS = 64e-5


class KB:
    def __init__(self, nc, n_dma_sems=48):
        self.nc = nc
        self.es = ExitStack()
        self.lists = {e: [] for e in ENGS}
        self.esem = {e: self.es.enter_context(nc.semaphore("s_" + e)) for e in ENGS if e != "sp"}
        self.ecnt = {e: 0 for e in ENGS}
        self.dsems = [self.es.enter_context(nc.semaphore("d%d" % i)) for i in range(n_dma_sems)]
        self.dcnt = [0] * n_dma_sems
        self.dnext = 0
        self.dnext_sw = 0
        self.waited = {e: {} for e in ENGS}
        self.lastw = {}
        self.readers = {}
        self.semobj = {}
        for e, s in self.esem.items():
            self.semobj[("e", e)] = s
        for i, s in enumerate(self.dsems):
            self.semobj[("d", i)] = s
        self.pes = None
        self.uid = 0
        self.last_func = None
        self.scr = None

    def phase_begin(self):
        self.pes = ExitStack()

    def sb(self, name, shape, dt, persistent=False):
        self.uid += 1
        st = self.es if (persistent or self.pes is None) else self.pes
        return st.enter_context(self.nc.sbuf_tensor("%s_%d" % (name, self.uid), list(shape), dt))

    def ps(self, name, shape, dt=F32):
        self.uid += 1
        return self.pes.enter_context(self.nc.psum_tensor("%s_%d" % (name, self.uid), list(shape), dt))

    def _need(self, eng, ev):
        if ev is None:
            return
        key, val = ev
        if eng == "pe" and key == ("e", "pe"):
            return
        if self.waited[eng].get(key, 0) >= val:
            return
        self.waited[eng][key] = val
        self.lists[eng].append(("wait", key, val))

    def _deps(self, eng, reads, writes):
        if os.environ.get("DBG_SWAP"):
            for w in writes:
                self._need(eng, self.lastw.get(w))
                for ev in self.readers.get(w, ()):
                    self._need(eng, ev)
        for r in reads:
            self._need(eng, self.lastw.get(r))
        for w in writes:
            self._need(eng, self.lastw.get(w))
            for ev in self.readers.get(w, ()):
                self._need(eng, ev)

    def _commit(self, ev, reads, writes):
        for r in reads:
            self.readers.setdefault(r, []).append(ev)
        for w in writes:
            self.lastw[w] = ev
            self.readers[w] = []

    def op(self, eng, fn, r=(), w=()):
        psr = [k for k in r if k.startswith("ps")]
        if psr:
            r = [k for k in r if not k.startswith("ps")]
            w = list(w) + psr
        self._deps(eng, r, w)
        self.ecnt[eng] += 1
        ev = (("e", eng), self.ecnt[eng])
        self.lists[eng].append(("op", fn, ("e", eng)))
        self._commit(ev, r, w)
        return ev

    def dma(self, q, out, in_, r=(), w=(), slow=False):
        self._deps(q, r, w)
        nsw = 8
        if q == "pool":
            i = self.dnext_sw
            self.dnext_sw = (self.dnext_sw + 1) % nsw
        else:
            i = nsw + self.dnext
            self.dnext = (self.dnext + 1) % (len(self.dsems) - nsw)
        if self.dcnt[i] > 0:
            self._need(q, (("d", i), 16 * self.dcnt[i]))
        self.dcnt[i] += 1
        ev = (("d", i), 16 * self.dcnt[i])
        if slow:
            fn = lambda e: e.dma_start(out=out, in_=in_, allow_slow_non_contiguous=True)
        else:
            fn = lambda e: e.dma_start(out=out, in_=in_)
        self.lists[q].append(("dma", fn, ("d", i)))
        self._commit(ev, r, w)
        return ev

    def barrier(self):
        evs = set(self.lastw.values())
        for rs in self.readers.values():
            evs.update(rs)
        for eng in ENGS:
            for ev in evs:
                self._need(eng, ev)
        self.lastw = {}
        self.readers = {}

    def sync_all(self):
        if os.environ.get("KEEP_BARRIERS"):
            self.barrier()
            self.emit()

    def phase_end(self):
        self.barrier()
        self.emit()
        self.pes.close()
        self.pes = None

    def emit(self):
        nc = self.nc
        semobj = self.semobj
        lists = self.lists

        nopmode = os.environ.get("DBG_NOP", "")

        attach = os.environ.get("DBG_ATTACH", "1") == "1"

        def run(engobj, items):
            pend = []
            for it in items:
                if it[0] == "wait":
                    pend.append(it)
                    continue
                last = None
                if attach and pend:
                    last = pend.pop()
                for w_ in pend:
                    engobj.wait_ge(semobj[w_[1]], w_[2])
                pend = []
                ins = it[1](engobj)
                if it[0] == "raw":
                    continue
                if last is not None:
                    ins = ins._wait_ge(semobj[last[1]], last[2])
                ins.then_inc(semobj[it[2]], 1 if it[0] == "op" else 16)
            for w_ in pend:
                engobj.wait_ge(semobj[w_[1]], w_[2])

        with nc.Block() as block:
            @block.tensor
            def _(e):
                run(e, lists["pe"])

            @block.vector
            def _(e):
                run(e, lists["dve"])

            @block.scalar
            def _(e):
                run(e, lists["act"])

            @block.gpsimd
            def _(e):
                run(e, lists["pool"])

            @block.sync
            def _(e):
                run(e, lists["sp"])
        self.lists = {e: [] for e in ENGS}

    def close(self):
        self.es.close()

    def mm(self, out, lhsT, rhs, start=True, stop=True, r=(), w=()):
        return self.op("pe", lambda e: e.matmul(out, lhsT=lhsT, rhs=rhs, start=start, stop=stop), r, w)

    def tr(self, out, in_, ident, r=(), w=()):
        return self.op("pe", lambda e: e.transpose(out, in_, ident), r, w)

    def act(self, out, in_, func, bias=None, scale=None, r=(), w=()):
        kw = {}
        if bias is not None:
            kw["bias"] = bias
        if scale is not None:
            kw["scale"] = scale
        return self.op("act", lambda e: e.activation(out, in_, func, **kw), r, w)

    def tt(self, eng, out, a, b, op, r=(), w=()):
        return self.op(eng, lambda e: e.tensor_tensor(out, a, b, op), r, w)

    def ts(self, eng, out, a, s1, s2, op0, op1=None, r=(), w=()):
        if op1 is None:
            return self.op(eng, lambda e: e.tensor_scalar(out, a, s1, None, op0), r, w)
        return self.op(eng, lambda e: e.tensor_scalar(out, a, s1, s2, op0, op1), r, w)

    def stt(self, eng, out, a, s, b, op0, op1, r=(), w=()):
        return self.op(eng, lambda e: e.scalar_tensor_tensor(out, a, s, b, op0, op1), r, w)

    def cp(self, eng, out, in_, r=(), w=()):
        if eng == "act":
            return self.op("act", lambda e: e.copy(out, in_), r, w)
        return self.op(eng, lambda e: e.tensor_copy(out, in_), r, w)

    def memset(self, eng, ap, v, r=(), w=()):
        return self.op(eng, lambda e: e.memset(ap, v), r, w)


def t5_bucket(n):
    import math
    exact = 16
    nf = np.maximum(n, 1).astype(np.float32)
    large = exact + (np.log(nf / exact) / math.log(2048 / exact) * (32 - exact)).astype(np.int32)
    large = np.minimum(large, 31)
    return np.where(n < exact, n, large).astype(np.int32)


DILS = (1, 4, 16)


def bias_index_tables():
    kj = np.arange(128)[:, None]
    qi = np.arange(128)[None, :]
    out = np.zeros((3, 128, 256), np.int64)
    for p, dil in enumerate(DILS):
        rel_c = qi - kj
        cur = np.where(rel_c >= 0, t5_bucket(np.clip(rel_c, 0, None) * dil), 32)
        rel_p = qi + 128 - kj
        prv = np.where(rel_p <= 128, t5_bucket(np.clip(rel_p, 0, None) * dil), 32)
        out[p, :, :128] = cur
        out[p, :, 128:] = prv
    return out


class Prog:
    pass


def build_program(dbg=None, stop_after=None, nb=NB):
    nc = bass.Bass("TRN2", target_bir_lowering=False)
    P = Prog()
    dt = lambda n, s, d=F32, k="ExternalInput": nc.dram_tensor(n, list(s), d, kind=k).ap()
    x = dt("x", [nb, S, D])
    pin = dt("p", [nb, S, 256])
    w_in = dt("w_in", [D, 3072])
    mu_rkv = dt("mu_rkv", [1536])
    mu_lora = dt("mu_lora", [3, D])
    w0 = dt("w0", [512]); a0 = dt("a0", [512])
    w_l1 = dt("w_lora1", [D, 64]); w_l2 = dt("w_lora2", [64, 512])
    a_l1 = dt("a_lora1", [D, 64]); a_l2 = dt("a_lora2", [64, 512])
    g_l1 = dt("g_lora1", [D, 128]); g_l2 = dt("g_lora2", [128, 512])
    k_k = dt("k_k", [512]); k_a = dt("k_a", [512]); r_k = dt("r_k", [512])
    lnx_g = dt("lnx_g", [512]); lnx_b = dt("lnx_b", [512])
    biasT = dt("biasT", [3, 8, 128, 256])
    w_o = dt("w_o", [D, D])
    ln1_g = dt("ln1_g", [D]); ln1_b = dt("ln1_b", [D])
    router = dt("router", [D, 36]); router_b = dt("router_b", [36])
    w_gate = dt("w_gate", [32, D, 512]); w_up = dt("w_up", [32, D, 512]); w_down = dt("w_down", [32, 512, D])
    ple_gate = dt("ple_gate", [D, D]); ple_proj = dt("ple_proj", [256, D])
    ln2_g = dt("ln2_g", [D]); ln2_b = dt("ln2_b", [D])
    out = dt("out", [nb, S, D], F32, "ExternalOutput")
    dbg_out = None
    if dbg is not None:
        dbg_out = dt("dbg", dbg[1], F32, "ExternalOutput")
    Wa = dt("Wa", [D, 3072], BF16, "Internal")
    Wb = dt("Wb", [D, 1536], BF16, "Internal")
    x1D = dt("x1D", [nb, S, D], F32, "Internal")
    mixD = dt("mixD", [nb, D, S], BF16, "Internal")
    x1TD = dt("x1TD", [nb, D, S], BF16, "Internal")
    gateD = dt("gateD", [nb, S, 32], F32, "Internal")

    kb = KB(nc)
    dq = ["sp", "sp"] if os.environ.get("DBG_SPONLY") else ["sp", "act"]
    dqi = [0]

    def q():
        dqi[0] ^= 1
        return dq[dqi[0]]

    identF = kb.sb("identF", [128, 128], F32, True)
    identB = kb.sb("identB", [128, 128], BF16, True)
    onesB = kb.sb("onesB", [128, 64], BF16, True)
    ones64 = kb.sb("ones64", [64, 64], F32, True)
    mean64 = kb.sb("mean64", [64, 64], F32, True)
    mSU = kb.sb("mSU", [128, 128], F32, True)
    mSL = kb.sb("mSL", [128, 128], F32, True)
    mIU = kb.sb("mIU", [128, 128], F32, True)
    m2SU = kb.sb("m2SU", [128, 256], F32, True)
    m2IU = kb.sb("m2IU", [128, 256], F32, True)
    BDones = kb.sb("BDones", [128, 128], F32, True)
    BDmean = kb.sb("BDmean", [128, 128], F32, True)
    csm = kb.sb("csm", [64, 256], F32, True)
    ones128 = kb.sb("ones128", [128, 128], F32, True)
    L1a = kb.sb("L1a", [128, 8, 256], BF16, True)
    L1b = kb.sb("L1b", [128, 8, 256], BF16, True)
    W2A2 = kb.sb("W2A2", [128, 512], BF16, True)
    G2 = kb.sb("G2", [128, 512], BF16, True)
    hv = kb.sb("hv", [128, 9, 4], F32, True)
    (V_W0, V_A0, V_KK, V_KA, V_RK, V_LG, V_LB, V_1MKA, V_X) = range(9)

    kb.scr = kb.sb("actscr", [128, 2], F32, True)
    kb.phase_begin()
    kb.memset("pool", kb.scr[:], 0.5, w=["actscr"])
    kb.memset("pool", identF[:], 0.0, w=["identF"])
    kb.op("pool", lambda e: e.affine_select(identF[:], identF[:], [[-1, 128]], ALU.not_equal, 1.0, base=0, channel_multiplier=1),
          r=["identF"], w=["identF"])
    kb.cp("dve", identB[:], identF[:], r=["identF"], w=["identB"])
    kb.memset("pool", onesB[:], 1.0, w=["onesB"])
    kb.memset("pool", ones64[:], 1.0, w=["ones64"])
    kb.memset("pool", mean64[:], 1.0 / 64.0, w=["mean64"])
    kb.memset("pool", mSU[:], 1.0, w=["mSU"])
    kb.op("pool", lambda e: e.affine_select(mSU[:], mSU[:], [[1, 128]], ALU.is_gt, 0.0, base=0, channel_multiplier=-1), r=["mSU"], w=["mSU"])
    kb.memset("pool", mSL[:], 1.0, w=["mSL"])
    kb.op("pool", lambda e: e.affine_select(mSL[:], mSL[:], [[-1, 128]], ALU.is_gt, 0.0, base=0, channel_multiplier=1), r=["mSL"], w=["mSL"])
    kb.memset("pool", mIU[:], 1.0, w=["mIU"])
    kb.op("pool", lambda e: e.affine_select(mIU[:], mIU[:], [[1, 128]], ALU.is_ge, 0.0, base=0, channel_multiplier=-1), r=["mIU"], w=["mIU"])
    kb.cp("pool", m2SU[:, 0:128], mSU[:], r=["mSU"], w=["m2SU"])
    kb.cp("pool", m2SU[:, 128:256], mSU[:], r=["mSU"], w=["m2SU"])
    kb.cp("pool", m2IU[:, 0:128], mIU[:], r=["mIU"], w=["m2IU"])
    kb.cp("pool", m2IU[:, 128:256], mIU[:], r=["mIU"], w=["m2IU"])
    for t_, val in ((BDones, 1.0), (BDmean, 1.0 / 64.0)):
        kb.memset("pool", t_[:], 0.0, w=["BD"])
        kb.memset("pool", t_[0:64, 0:64], val, r=["BD"], w=["BD"])
        kb.memset("pool", t_[64:128, 64:128], val, r=["BD"], w=["BD"])
    kb.memset("pool", csm[:], 0.0, w=["csm"])
    kb.memset("pool", ones128[:], 1.0, w=["ones128"])
    kb.ts("dve", csm[:, 0:64], identF[0:64, 0:64], -1.0 / 64.0, None, ALU.add, r=["identF", "csm"], w=["csm"])
    kb.ts("dve", csm[:, 192:256], identF[0:64, 0:64], -1.0 / 64.0, None, ALU.add, r=["identF", "csm"], w=["csm"])
    for i, v in ((V_W0, w0), (V_A0, a0), (V_KK, k_k), (V_KA, k_a), (V_RK, r_k), (V_LG, lnx_g), (V_LB, lnx_b)):
        kb.dma("sp", hv[:, i, :], v.rearrange("(h p) -> p h", p=128), w=["hv"], slow=True)
    kb.ts("dve", hv[:, V_1MKA, :], hv[:, V_KA, :], -1.0, 1.0, ALU.mult, ALU.add, r=["hv"], w=["hv"])
    kb.dma("pool", W2A2[0:64, :], w_l2[:, :], w=["W2A2"])
    kb.dma("pool", W2A2[64:128, :], a_l2[:, :], w=["W2A2"])
    kb.dma("pool", G2[:], g_l2[:, :], w=["G2"])
    mul = kb.sb("mul", [128, 3, 8], F32)
    omul = kb.sb("omul", [128, 3, 8], F32)
    for j in range(3):
        kb.dma("sp", mul[:, j, :], mu_lora[j].rearrange("(c p) -> p c", p=128), w=["mul"], slow=True)
    kb.ts("dve", omul[:], mul[:], -1.0, 1.0, ALU.mult, ALU.add, r=["mul"], w=["omul"])
    l1t = kb.sb("l1t", [128, 8, 256], F32)
    kb.dma("sp", l1t[:, :, 0:64], w_l1.rearrange("(c p) j -> p c j", p=128), w=["l1t"])
    kb.dma(q(), l1t[:, :, 64:128], a_l1.rearrange("(c p) j -> p c j", p=128), w=["l1t"])
    kb.dma("sp", l1t[:, :, 128:256], g_l1.rearrange("(c p) j -> p c j", p=128), w=["l1t"])
    for c in range(8):
        for j, (lo, hi) in enumerate(((0, 64), (64, 128), (128, 256))):
            kb.ts("dve", L1a[:, c, lo:hi], l1t[:, c, lo:hi], omul[:, j, c:c + 1], None, ALU.mult, r=["l1t", "omul"], w=["L1a"])
            kb.ts("pool", L1b[:, c, lo:hi], l1t[:, c, lo:hi], mul[:, j, c:c + 1], None, ALU.mult, r=["l1t", "mul"], w=["L1b"])
    mubc = kb.sb("mubc", [128, 1536], F32)
    omubc = kb.sb("omubc", [128, 1536], F32)
    kb.dma("sp", mubc[:], mu_rkv.partition_broadcast(128), w=["mubc"])
    kb.ts("dve", omubc[:], mubc[:], -1.0, 1.0, ALU.mult, ALU.add, r=["mubc"], w=["omubc"])
    wt = [kb.sb("wt%d" % i, [128, 3072], F32) for i in range(2)]
    wa = [kb.sb("wa%d" % i, [128, 3072], BF16) for i in range(2)]
    wb = [kb.sb("wb%d" % i, [128, 1536], BF16) for i in range(2)]
    for c in range(8):
        i = c % 2
        kb.dma(q(), wt[i][:], w_in[c * 128:(c + 1) * 128, :], w=["wt%d" % i])
        kb.tt("dve", wa[i][:, 0:1536], wt[i][:, 0:1536], omubc[:], ALU.mult, r=["wt%d" % i, "omubc"], w=["wa%d" % i])
        kb.cp("act", wa[i][:, 1536:3072], wt[i][:, 1536:3072], r=["wt%d" % i], w=["wa%d" % i])
        kb.tt("pool", wb[i][:], wt[i][:, 0:1536], mubc[:], ALU.mult, r=["wt%d" % i, "mubc"], w=["wb%d" % i])
        kb.dma(q(), Wa[c * 128:(c + 1) * 128, :], wa[i][:], r=["wa%d" % i], w=["WaD"])
        kb.dma(q(), Wb[c * 128:(c + 1) * 128, :], wb[i][:], r=["wb%d" % i], w=["WbD"])
    kb.phase_end()

    def dump(ap_sb, key, dram_ap):
        kb.dma("sp", dram_ap, ap_sb, r=[key], w=["dbgout"])

    for b in range(nb):
        bes = ExitStack()
        hT = bes.enter_context(nc.sbuf_tensor("hT_b%d" % b, [128, 8, S + 1], BF16))

        kb.phase_begin()
        l1wa = bes.enter_context(nc.sbuf_tensor("l1wa_b%d" % b, [128, S], BF16))
        l1g = bes.enter_context(nc.sbuf_tensor("l1g_b%d" % b, [128, S], BF16))
        xt = [kb.sb("xt%d" % i, [128, D], F32) for i in range(4)]
        psA = [kb.ps("psA%d" % i, [128, 512]) for i in range(4)]
        kb.memset("pool", hT[:, :, 0:1], 0.0, w=["hT"])
        pi = 0
        for t in range(16):
            i = t % 4
            kb.dma(q(), xt[i][:], x[b, t * 128:(t + 1) * 128, :], w=["xt%d" % i])
            for half in range(2):
                pk = "psA%d" % (pi % 4)
                ps = psA[pi % 4]
                pi += 1
                for cc in range(4):
                    c = half * 4 + cc
                    kb.tr(ps[:, cc * 128:(cc + 1) * 128], xt[i][:, c * 128:(c + 1) * 128], identF[:],
                          r=["xt%d" % i, "identF"], w=[pk])
                eng = "dve" if half == 0 else "act"
                kb.cp(eng, hT[:, half * 4:half * 4 + 4, 1 + t * 128:1 + (t + 1) * 128],
                      ps[:].rearrange("p (c t) -> p c t", t=128), r=[pk], w=["hT"])
        for tt_ in range(4):
            ts_ = slice(tt_ * 512, (tt_ + 1) * 512)
            for oc in range(2):
                pk = "psA%d" % (pi % 4)
                ps = psA[pi % 4]
                pi += 1
                for c in range(8):
                    kb.mm(ps[:], L1a[:, c, oc * 128:(oc + 1) * 128], hT[:, c, 1 + tt_ * 512:1 + (tt_ + 1) * 512],
                          start=(c == 0), stop=False, r=["hT", "L1a"], w=[pk])
                    kb.mm(ps[:], L1b[:, c, oc * 128:(oc + 1) * 128], hT[:, c, tt_ * 512:(tt_ + 1) * 512],
                          start=False, stop=(c == 7), r=["hT", "L1b"], w=[pk])
                if oc == 0:
                    kb.act(l1wa[0:64, ts_], ps[0:64, :], AF.Tanh, r=[pk], w=["l1wa"])
                    kb.cp("dve", l1wa[64:128, ts_], ps[64:128, :], r=[pk], w=["l1wa"])
                else:
                    kb.act(l1g[:, ts_], ps[:], AF.Sigmoid, r=[pk], w=["l1g"])
        if dbg and dbg[0] == "hT" and b == 0:
            tmp = kb.sb("dbgtmp", [128, S], F32)
            kb.cp("dve", tmp[:], hT[:, 3, 1:S + 1], r=["hT"], w=["dbgtmp"])
            dump(tmp[:], "dbgtmp", dbg_out[:, :])
        kb.phase_end()
        if stop_after == "A":
            bes.close()
            break

        for hg in range(0 if os.environ.get('DBG_SKIPB') else 2):
            kb.phase_begin()
            QT = kb.sb("QT", [128, 2, S], BF16)
            KT = kb.sb("KT", [128, 2, S], BF16)
            V3 = kb.sb("V3", [128, 3, 16, 4, 65], BF16)
            kb.memset("pool", V3[:, :, :, :, 64:65], 1.0, w=["V3"])
            wq = kb.sb("wq", [128, 8, 768], BF16)
            numA = kb.sb("numA", [65, S], F32)
            NL = 5
            PT = [kb.sb("PT%d" % i, [128, 256], BF16) for i in range(NL)]
            yat = [kb.sb("yat%d" % i, [64, S], BF16) for i in range(2)]
            biasS = kb.sb("biasS", [128, 24, 256], BF16)
            kb.dma("pool", biasS[:], biasT.rearrange("p h k q -> k (p h) q"), w=["biasS"])
            pbk = [kb.ps("psK%d" % i, [128, 512]) for i in range(8)]
            psP = pbk[0:2]
            psL = pbk[0:NL]
            psN = pbk[NL:8]
            for j, off in enumerate((1536, 2048, 2560)):
                kb.dma(q(), wq[:, :, j * 256:(j + 1) * 256],
                       Wa[:, off + hg * 256: off + (hg + 1) * 256].rearrange("(c p) f -> p c f", p=128),
                       r=["WaD"], w=["wq"])
            pi = 0
            for j, dst in ((0, QT), (1, KT)):
                for fc in range(2):
                    for tt_ in range(4):
                        pk = "psK%d" % (pi % 2)
                        ps = psP[pi % 2]
                        pi += 1
                        for c in range(8):
                            kb.mm(ps[:], wq[:, c, j * 256 + fc * 128: j * 256 + (fc + 1) * 128],
                                  hT[:, c, 1 + tt_ * 512:1 + (tt_ + 1) * 512], start=(c == 0), stop=(c == 7),
                                  r=["hT", "wq"], w=[pk])
                        if j == 0:
                            kb.act(dst[:, fc, tt_ * 512:(tt_ + 1) * 512], ps[:], AF.Copy, scale=0.125, r=[pk], w=["QT"])
                        else:
                            kb.cp("dve", dst[:, fc, tt_ * 512:(tt_ + 1) * 512], ps[:], r=[pk], w=["KT"])
            for p_, dil in enumerate(DILS):
                for ti in range(16):
                    nbk = 16 // dil
                    rcls, m = ti // nbk, ti % nbk
                    pk = "psK%d" % (pi % 2)
                    ps = psP[pi % 2]
                    pi += 1
                    st = 1 + dil * 128 * m + rcls
                    for c in range(8):
                        kb.mm(ps[:, 0:256], hT[:, c, st: st + dil * 127 + 1: dil], wq[:, c, 512:768],
                              start=(c == 0), stop=(c == 7), r=["hT", "wq"], w=[pk])
                    kb.cp("dve" if ti % 2 else "act", V3[:, p_, ti, :, 0:64], ps[:, 0:256].rearrange("p (h c) -> p h c", c=64), r=[pk], w=["V3"])
            for hl in range(4):
                h = hg * 4 + hl
                fc, pb = hl // 2, 64 * (hl % 2)
                kb.memset("pool", numA[:], 0.0, w=["numA"])
                units = []
                for p_, dil in enumerate(DILS):
                    nbk = 16 // dil
                    for rcls in range(dil):
                        for m in range(nbk):
                            units.append((p_, dil, rcls, m, nbk))
                pend = []
                ui = 0

                def second(u, ui_):
                    p_, dil, rcls, m, nbk = u
                    nq = 256 if m + 1 < nbk else 128
                    ptk = "PT%d" % (ui_ % NL)
                    pt = PT[ui_ % NL]
                    pnk = "psK%d" % (NL + ui_ % 3)
                    pn = psN[ui_ % 3]
                    ti = rcls * nbk + m
                    kb.mm(pn[0:65, 0:nq], V3[:, p_, ti, hl, :], pt[:, 0:nq], r=[ptk, "V3"], w=[pnk])
                    st = dil * 128 * m + rcls
                    sl = slice(st, st + dil * (nq - 1) + 1, dil)
                    kb.tt("dve", numA[:, sl], numA[:, sl], pn[0:65, 0:nq], ALU.add, r=[pnk, "numA"], w=["numA"])

                for u in units:
                    p_, dil, rcls, m, nbk = u
                    nq = 256 if m + 1 < nbk else 128
                    plk = "psK%d" % (ui % NL)
                    pl = psL[ui % NL]
                    ptk = "PT%d" % (ui % NL)
                    pt = PT[ui % NL]
                    st = dil * 128 * m + rcls
                    kb.mm(pl[:, 0:nq], KT[pb:pb + 64, fc, st: st + dil * 127 + 1: dil],
                          QT[pb:pb + 64, fc, st: st + dil * (nq - 1) + 1: dil], start=True, stop=False,
                          r=["KT", "QT"], w=[plk])
                    kb.mm(pl[:, 0:nq], identB[:], biasS[:, p_ * 8 + h, 0:nq], start=False, stop=True,
                          r=["identB", "biasS"], w=[plk])
                    kb.act(pt[:, 0:nq], pl[:, 0:nq], AF.Exp, r=[plk], w=[ptk])
                    pend.append((u, ui))
                    ui += 1
                    if len(pend) > NL - 1:
                        second(*pend.pop(0))
                while pend:
                    second(*pend.pop(0))
                kb.op("dve", lambda e: e.reciprocal(numA[64:65, :], numA[64:65, :]), r=["numA"], w=["numA"])
                for tq in range(4):
                    pb_, pbk_ = pbk[NL + tq % 3], "psK%d" % (NL + tq % 3)
                    kb.mm(pb_[0:64, :], ones128[64:65, 0:64], numA[64:65, tq * 512:(tq + 1) * 512], r=["numA", "ones128"], w=[pbk_])
                    kb.tt("dve", yat[hl % 2][:, tq * 512:(tq + 1) * 512], numA[0:64, tq * 512:(tq + 1) * 512], pb_[0:64, :], ALU.mult,
                          r=["numA", pbk_], w=["yat%d" % (hl % 2)])
                kb.dma(q(), mixD[b, 512 + h * 64:512 + (h + 1) * 64, :], yat[hl % 2][:], r=["yat%d" % (hl % 2)], w=["mixD"])
            kb.phase_end()
        if stop_after == "B":
            bes.close()
            break

        C0 = float(np.exp(-0.5))
        for hp in range(int(os.environ.get('DBG_NHP', '4'))):
            kb.phase_begin()
            Tn = ("r", "k", "v", "a", "kk", "lw", "cum", "x")
            T = {n: kb.sb("T_" + n, [128, S], F32) for n in Tn}
            T_g = kb.sb("T_g", [128, S], BF16)
            T_bon = kb.sb("T_bon", [128, S], BF16)
            Hs = [kb.sb("Hs%d" % i, [64, 17, 64], F32) for i in range(2)]
            psAll = [kb.ps("psC%d" % i, [128, 512]) for i in range(8)]
            cnt = {"b": 0, "s": 0, "e": 0}

            def big():
                i = cnt["b"] % 3
                cnt["b"] += 1
                return psAll[i], "psC%d" % i

            def sml():
                i = cnt["s"] % 8
                cnt["s"] += 1
                return psAll[i], "psC%d" % i

            def ev2():
                cnt["e"] += 1
                return "dve" if cnt["e"] % 2 else "act"

            K_ = lambda n, tq: "T_%s.%d" % (n, tq)
            wres = ExitStack()
            kb.uid += 1
            wr = wres.enter_context(nc.sbuf_tensor("wr_%d" % kb.uid, [128, 8, 6, 128], BF16))
            for j in range(3):
                kb.dma(q(), wr[:, :, j, :], Wa[:, j * 512 + hp * 128: j * 512 + (hp + 1) * 128].rearrange("(c p) f -> p c f", p=128),
                       r=["WaD"], w=["wr"])
                kb.dma(q(), wr[:, :, 3 + j, :], Wb[:, j * 512 + hp * 128: j * 512 + (hp + 1) * 128].rearrange("(c p) f -> p c f", p=128),
                       r=["WbD"], w=["wr"])
            hcol = lambda i: hv[:, i, hp:hp + 1]
            fcols = slice(hp * 128, (hp + 1) * 128)
            TQ = [(tq, slice(tq * 512, (tq + 1) * 512)) for tq in range(4)]
            for tq, ts_ in TQ:
                for j, n in enumerate(("r", "k", "v")):
                    ps, pk = big()
                    for c in range(8):
                        kb.mm(ps[:], wr[:, c, j, :], hT[:, c, 1 + tq * 512:1 + (tq + 1) * 512], start=(c == 0), stop=False,
                              r=["hT", "wr"], w=[pk])
                        kb.mm(ps[:], wr[:, c, 3 + j, :], hT[:, c, tq * 512:(tq + 1) * 512], start=False, stop=(c == 7),
                              r=["hT", "wr"], w=[pk])
                    kb.cp(ev2(), T[n][:, ts_], ps[:], r=[pk], w=[K_(n, tq)])
                ps, pk = big()
                kb.mm(ps[:], W2A2[0:64, fcols], l1wa[0:64, ts_], r=["l1wa", "W2A2"], w=[pk])
                kb.act(T["lw"][:, ts_], ps[:], AF.Sigmoid, bias=hcol(V_W0), r=[pk, "hv"], w=[K_("lw", tq)])
                ps, pk = big()
                kb.mm(ps[:], W2A2[64:128, fcols], l1wa[64:128, ts_], r=["l1wa", "W2A2"], w=[pk])
                kb.act(T["a"][:, ts_], ps[:], AF.Sigmoid, bias=hcol(V_A0), r=[pk, "hv"], w=[K_("a", tq)])
                ps, pk = big()
                kb.mm(ps[:], G2[:, fcols], l1g[:, ts_], r=["l1g", "G2"], w=[pk])
                kb.cp("dve", T_g[:, ts_], ps[:], r=[pk], w=[K_("g", tq)])
            kb.sync_all()
            wres.close()
            for tq, ts_ in TQ:
                kb.ts("dve", T["kk"][:, ts_], T["k"][:, ts_], hcol(V_KK), None, ALU.mult, r=[K_("k", tq), "hv"], w=[K_("kk", tq)])
                kb.tt("dve", T["x"][:, ts_], T["kk"][:, ts_], T["kk"][:, ts_], ALU.mult, r=[K_("kk", tq)], w=[K_("x", tq)])
                ps, pk = big()
                kb.mm(ps[:], BDones[:], T["x"][:, ts_], r=[K_("x", tq), "BD"], w=[pk])
                kb.ts("dve", T["x"][:, ts_], ps[:], 1e-24, None, ALU.max, r=[pk], w=[K_("x", tq)])
            kb.sync_all()
            for tq, ts_ in TQ:
                kb.act(T["x"][:, ts_], T["x"][:, ts_], AF.Sqrt, r=[K_("x", tq)], w=[K_("x", tq)])
            kb.sync_all()
            for tq, ts_ in TQ:
                kb.op("dve", lambda e, ts_=ts_: e.reciprocal(T["x"][:, ts_], T["x"][:, ts_]), r=[K_("x", tq)], w=[K_("x", tq)])
                kb.tt("dve", T["kk"][:, ts_], T["kk"][:, ts_], T["x"][:, ts_], ALU.mult, r=[K_("kk", tq), K_("x", tq)], w=[K_("kk", tq)])
                kb.ts("dve", T["x"][:, ts_], T["a"][:, ts_], hcol(V_KA), hcol(V_1MKA), ALU.mult, ALU.add,
                      r=[K_("a", tq), "hv"], w=[K_("x", tq)])
                kb.tt("dve", T["k"][:, ts_], T["k"][:, ts_], T["x"][:, ts_], ALU.mult, r=[K_("k", tq), K_("x", tq)], w=[K_("k", tq)])
                kb.stt("dve", T["x"][:, ts_], T["r"][:, ts_], hcol(V_RK), T["k"][:, ts_], ALU.mult, ALU.mult,
                       r=[K_("r", tq), K_("k", tq), "hv"], w=[K_("x", tq)])
                ps, pk = big()
                kb.mm(ps[:], BDones[:], T["x"][:, ts_], r=[K_("x", tq), "BD"], w=[pk])
                kb.tt("dve", T_bon[:, ts_], ps[:], T["v"][:, ts_], ALU.mult, r=[pk, K_("v", tq)], w=[K_("bon", tq)])
                for c4 in range(4):
                    cs_ = slice(tq * 512 + c4 * 128, tq * 512 + (c4 + 1) * 128)
                    kb.op("dve", lambda e, cs_=cs_: e.tensor_tensor_scan(T["cum"][:, cs_], ones128[:, 0:128], T["lw"][:, cs_], 0.0, ALU.mult, ALU.add),
                          r=[K_("lw", tq), "ones128"], w=[K_("cum", tq)])
                kb.tt("dve", T["x"][:, ts_], T["cum"][:, ts_], T["lw"][:, ts_], ALU.subtract, r=[K_("cum", tq), K_("lw", tq)], w=[K_("x", tq)])
            kb.sync_all()
            for tq, ts_ in TQ:
                kb.act(T["x"][:, ts_], T["x"][:, ts_], AF.Exp, scale=-C0, r=[K_("x", tq)], w=[K_("x", tq)])
                kb.act(T["lw"][:, ts_], T["cum"][:, ts_], AF.Exp, scale=C0, r=[K_("cum", tq)], w=[K_("lw", tq)])
                kb.act(T["cum"][:, ts_], T["cum"][:, ts_], AF.Exp, scale=-C0, r=[K_("cum", tq)], w=[K_("cum", tq)])
            kb.sync_all()
            for tq, ts_ in TQ:
                kb.stt("dve", T["x"][:, ts_], T["kk"][:, ts_], -1.0, T["x"][:, ts_], ALU.mult, ALU.mult,
                       r=[K_("kk", tq), K_("x", tq)], w=[K_("x", tq)])
                kb.tt("dve", T["kk"][:, ts_], T["kk"][:, ts_], T["a"][:, ts_], ALU.mult, r=[K_("kk", tq), K_("a", tq)], w=[K_("kk", tq)])
                kb.tt("dve", T["kk"][:, ts_], T["kk"][:, ts_], T["lw"][:, ts_], ALU.mult, r=[K_("kk", tq), K_("lw", tq)], w=[K_("kk", tq)])
                kb.tt("dve", T["k"][:, ts_], T["k"][:, ts_], T["lw"][:, ts_], ALU.mult, r=[K_("k", tq), K_("lw", tq)], w=[K_("k", tq)])
                kb.tt("dve", T["r"][:, ts_], T["r"][:, ts_], T["cum"][:, ts_], ALU.mult, r=[K_("r", tq), K_("cum", tq)], w=[K_("r", tq)])
            kb.sync_all()
            At, Bt, Kt, Rt, Gam, Vt = T["x"], T["kk"], T["k"], T["r"], T["cum"], T["v"]
            YT = [T["lw"], T["a"]]
            ytn = ["lw", "a"]
            KW = int(os.environ.get("SCAN_WAYS", "6"))
            PPs = [[kb.sb("PP%d_%d" % (sl, i), [128, 256], BF16) for i in range(7)] for sl in range(KW)]
            XBs = [[kb.sb("XB%d_%d" % (sl, i), [128, 128], BF16) for i in range(2)] for sl in range(KW)]
            NAs = [kb.sb("NA%d" % sl, [128, 256], F32) for sl in range(KW)]
            ARs = [kb.sb("AR%d" % sl, [128, 256], F32) for sl in range(KW)]
            XXs = [[kb.sb("XX%d_%d" % (sl, i), [128, 128], F32) for i in range(4)] for sl in range(KW)]
            tokms = [kb.sb("tokm%d" % sl, [128, 4, 64], F32) for sl in range(KW)]
            blkls = [kb.sb("blkl%d" % sl, [128, 2, 128], F32) for sl in range(KW)]
            RhTs = [kb.sb("RhT%d" % sl, [64, 128], F32) for sl in range(KW)]
            YiTs = [kb.sb("YiT%d" % sl, [64, 128], F32) for sl in range(KW)]
            Gts = [kb.sb("Gt%d" % sl, [64, 64], F32) for sl in range(KW)]
            Fpps = [kb.sb("Fpp%d" % sl, [64, 64], F32) for sl in range(KW)]
            for e_ in range(2):
                kb.memset("pool", Hs[e_][:, 0, :], 0.0, w=["Hs%d_0" % e_])

            def unit(e_, c, sl):
                pb = 64 * e_
                pbs = slice(pb, pb + 64)
                idp = identF[pbs, pb:pb + 64]
                PP, NA, AR, XX, tokm, blkl = PPs[sl], NAs[sl], ARs[sl], XXs[sl], tokms[sl], blkls[sl]
                XB = XBs[sl]
                RhT, YiT, Gt, Fpp = RhTs[sl], YiTs[sl], Gts[sl], Fpps[sl]
                P2 = "_%d" % sl
                tq = c // 4
                cs_ = slice(c * 128, (c + 1) * 128)
                gl = Gam[pbs, c * 128 + 127:c * 128 + 128]
                kb.ts("dve", blkl[pbs, 0, :], Bt[pbs, cs_], gl, None, ALU.mult, r=[K_("kk", tq), K_("cum", tq)], w=["blkl" + P2])
                kb.act(blkl[pbs, 1, :], Kt[pbs, cs_], AF.Copy, scale=gl, r=[K_("k", tq), K_("cum", tq)], w=["blkl" + P2])
                ps, pk = sml()
                kb.tr(ps[:, 0:64], At[pbs, cs_], idp, r=[K_("x", tq), "identF"], w=[pk])
                kb.tr(ps[:, 64:128], blkl[pbs, 0, :], idp, r=["blkl" + P2, "identF"], w=[pk])
                kb.tr(ps[:, 128:192], blkl[pbs, 1, :], idp, r=["blkl" + P2, "identF"], w=[pk])
                kb.tr(ps[:, 192:256], Vt[pbs, cs_], idp, r=[K_("v", tq), "identF"], w=[pk])
                kb.cp("act", tokm[:].rearrange("p a b -> p (a b)"), ps[:, 0:256], r=[pk], w=["tokm" + P2])
                yield
                ps, pk = sml()
                kb.mm(ps[:, 0:128], Bt[pbs, cs_], At[pbs, cs_], r=[K_("kk", tq), K_("x", tq)], w=[pk])
                kb.mm(ps[:, 128:256], Kt[pbs, cs_], At[pbs, cs_], r=[K_("k", tq), K_("x", tq)], w=[pk])
                kb.mm(ps[:, 256:384], At[pbs, cs_], Bt[pbs, cs_], r=[K_("kk", tq), K_("x", tq)], w=[pk])
                kb.tt("dve", NA[:], ps[:, 0:256], m2SU[:], ALU.mult, r=[pk, "m2SU"], w=["NA" + P2])
                kb.tt("dve", PP[0][:, 0:128], ps[:, 256:384], mSL[:], ALU.mult, r=[pk, "mSL"], w=["PP0n" + P2])
                kb.cp("act", PP[0][:, 128:256], NA[:, 0:128], r=["NA" + P2], w=["PP0t" + P2])
                ps, pk = sml()
                kb.mm(ps[:, 0:128], Bt[pbs, cs_], Rt[pbs, cs_], r=[K_("kk", tq), K_("r", tq)], w=[pk])
                kb.mm(ps[:, 128:256], Kt[pbs, cs_], Rt[pbs, cs_], r=[K_("k", tq), K_("r", tq)], w=[pk])
                kb.tt("dve", AR[:], ps[:, 0:256], m2IU[:], ALU.mult, r=[pk, "m2IU"], w=["AR" + P2])
                yield
                xi = 0
                ps, pk = sml()
                kb.mm(ps[:, 0:64], NA[:, 128:256], tokm[:, 3, :], r=["NA" + P2, "tokm" + P2], w=[pk])
                kb.cp("act", XX[0][:, 0:64], tokm[:, 0, :], r=["tokm" + P2], w=["XX0" + P2])
                kb.cp("act", XX[0][:, 64:128], ps[:, 0:64], r=[pk], w=["XX0" + P2])
                kb.cp("act", XB[0][:], XX[0][:], r=["XX0" + P2], w=["XB0" + P2])
                yield
                for i in range(7):
                    ps, pk = sml()
                    kb.mm(ps[:, 0:128], PP[i][:, 128:256], XB[i % 2][:], r=["PP%dt" % i + P2, "XB%d" % (i % 2) + P2], w=[pk])
                    if i < 6:
                        ps2, pk2 = sml()
                        if i < 5:
                            kb.mm(ps2[:, 0:128], PP[i][:, 128:256], PP[i][:, 0:128], r=["PP%dt" % i + P2, "PP%dn" % i + P2], w=[pk2])
                        kb.mm(ps2[:, 128:256], PP[i][:, 0:128], PP[i][:, 128:256], r=["PP%dt" % i + P2, "PP%dn" % i + P2], w=[pk2])
                        if i < 5:
                            kb.cp("act", PP[i + 1][:], ps2[:, 0:256], r=[pk2], w=["PP%dn" % (i + 1) + P2, "PP%dt" % (i + 1) + P2])
                        else:
                            kb.cp("act", PP[i + 1][:, 128:256], ps2[:, 128:256], r=[pk2], w=["PP%dt" % (i + 1) + P2])
                    nx = (xi + 1) % 4
                    kb.tt("dve", XX[nx][:], ps[:, 0:128], XX[xi][:], ALU.add, r=[pk, "XX%d" % xi + P2], w=["XX%d" % nx + P2])
                    if i < 6:
                        kb.cp("act", XB[(i + 1) % 2][:], XX[nx][:], r=["XX%d" % nx + P2], w=["XB%d" % ((i + 1) % 2) + P2])
                    xi = nx
                    yield
                Xf = XX[xi]
                xk = "XX%d" % xi + P2
                ps, pk = sml()
                kb.mm(ps[0:64, 0:128], Xf[:, 0:64], AR[:, 0:128], r=[xk, "AR" + P2], w=[pk])
                kb.mm(ps[0:64, 256:320], Xf[:, 0:64], tokm[:, 1, :], r=[xk, "tokm" + P2], w=[pk])
                ps2, pk2 = sml()
                kb.mm(ps2[0:64, 128:256], Xf[:, 64:128], AR[:, 0:128], start=True, stop=False, r=[xk, "AR" + P2], w=[pk2])
                kb.mm(ps2[0:64, 128:256], tokm[:, 3, :], AR[:, 128:256], start=False, stop=True, r=["tokm" + P2, "AR" + P2], w=[pk2])
                kb.mm(ps2[0:64, 320:384], tokm[:, 1, :], Xf[:, 64:128], start=True, stop=False, r=[xk, "tokm" + P2], w=[pk2])
                kb.mm(ps2[0:64, 320:384], tokm[:, 2, :], tokm[:, 3, :], start=False, stop=True, r=["tokm" + P2], w=[pk2])
                kb.tt("dve", RhT[:], ps[0:64, 0:128], Rt[pbs, cs_], ALU.add, r=[pk, K_("r", tq)], w=["RhT" + P2])
                kb.stt("dve", Gt[:], idp, gl, ps[0:64, 256:320], ALU.mult, ALU.add, r=[pk, "identF", K_("cum", tq)], w=["Gt" + P2])
                kb.cp("act", YiT[:], ps2[0:64, 128:256], r=[pk2], w=["YiT" + P2])
                kb.cp("act", Fpp[:], ps2[0:64, 320:384], r=[pk2], w=["Fpp" + P2])
                yield
                ps, pk = sml()
                HK = "Hs%d_%d" % (e_, c)
                kb.mm(ps[0:64, 0:128], Hs[e_][:, c, :], RhT[:], r=[HK, "RhT" + P2], w=[pk])
                kb.mm(ps[0:64, 128:192], Gt[:], Hs[e_][:, c, :], r=[HK, "Gt" + P2], w=[pk])
                kb.tt("dve", Hs[e_][:, c + 1, :], ps[0:64, 128:192], Fpp[:], ALU.add, r=[pk, "Fpp" + P2], w=["Hs%d_%d" % (e_, c + 1)])
                kb.tt("dve", YT[e_][0:64, cs_], ps[0:64, 0:128], YiT[:], ALU.add, r=[pk, "YiT" + P2], w=[K_(ytn[e_], tq)])
                yield

            todo = [(e_, c) for c in range(int(os.environ.get('DBG_NC', '16'))) for e_ in range(2)]
            active = [None] * KW
            while todo or any(g is not None for g in active):
                started = False
                for sl in range(KW):
                    if active[sl] is None and todo and not started:
                        e_, c = todo.pop(0)
                        active[sl] = unit(e_, c, sl)
                        started = True
                    if active[sl] is not None:
                        try:
                            next(active[sl])
                        except StopIteration:
                            active[sl] = None
            kb.sync_all()
            for tq, ts_ in TQ:
                ps, pk = big()
                kb.mm(ps[:], csm[:, 0:128], YT[0][0:64, ts_], start=True, stop=False, r=[K_("lw", tq), "csm"], w=[pk])
                kb.mm(ps[:], csm[:, 128:256], YT[1][0:64, ts_], start=False, stop=True, r=[K_("a", tq), "csm"], w=[pk])
                kb.cp("dve", T["x"][:, ts_], ps[:], r=[pk], w=[K_("x", tq)])
                kb.tt("dve", T["kk"][:, ts_], T["x"][:, ts_], T["x"][:, ts_], ALU.mult, r=[K_("x", tq)], w=[K_("kk", tq)])
                ps, pk = big()
                kb.mm(ps[:], BDmean[:], T["kk"][:, ts_], r=[K_("kk", tq), "BD"], w=[pk])
                kb.ts("dve", T["kk"][:, ts_], ps[:], LNX_EPS, None, ALU.add, r=[pk], w=[K_("kk", tq)])
            kb.sync_all()
            for tq, ts_ in TQ:
                kb.act(T["kk"][:, ts_], T["kk"][:, ts_], AF.Sqrt, r=[K_("kk", tq)], w=[K_("kk", tq)])
            kb.sync_all()
            for tq, ts_ in TQ:
                kb.op("dve", lambda e, ts_=ts_: e.reciprocal(T["kk"][:, ts_], T["kk"][:, ts_]), r=[K_("kk", tq)], w=[K_("kk", tq)])
                kb.tt("dve", T["x"][:, ts_], T["x"][:, ts_], T["kk"][:, ts_], ALU.mult, r=[K_("x", tq), K_("kk", tq)], w=[K_("x", tq)])
                kb.ts("dve", T["x"][:, ts_], T["x"][:, ts_], hcol(V_LG), hcol(V_LB), ALU.mult, ALU.add, r=[K_("x", tq), "hv"], w=[K_("x", tq)])
                kb.tt("dve", T["x"][:, ts_], T["x"][:, ts_], T_bon[:, ts_], ALU.add, r=[K_("x", tq), K_("bon", tq)], w=[K_("x", tq)])
                kb.tt("dve", T_bon[:, ts_], T["x"][:, ts_], T_g[:, ts_], ALU.mult, r=[K_("x", tq), K_("g", tq), K_("bon", tq)], w=[K_("bon", tq)])
                kb.dma(q(), mixD[b, hp * 128:(hp + 1) * 128, ts_], T_bon[:, ts_], r=[K_("bon", tq)], w=["mixD"])
            kb.phase_end()
        if stop_after == "C":
            bes.close()
            break
        kb.phase_begin()
        Wo = kb.sb("Wo", [128, 8, D], BF16)
        mixT = kb.sb("mixT", [128, 8, S], BF16)
        kb.dma("sp", mixT[:, 0:4, :], mixD[b, 0:512, :].rearrange("(c p) t -> p c t", p=128), r=["mixD"], w=["mixT"])
        kb.dma("act", mixT[:, 4:8, :], mixD[b, 512:1024, :].rearrange("(c p) t -> p c t", p=128), r=["mixD"], w=["mixT"])
        Wr = kb.sb("Wr", [128, 8, 36], F32)
        rbb = kb.sb("rbb", [128, 36], F32)
        g1b = kb.sb("g1b", [128, D], F32)
        b1b = kb.sb("b1b", [128, D], F32)
        xt = [kb.sb("xtD%d" % i, [128, D], F32) for i in range(4)]
        yt = [kb.sb("ytD%d" % i, [128, D], F32) for i in range(4)]
        sqt2 = [kb.sb("sqD%d" % i, [128, D], F32) for i in range(4)]
        x1Tb = [kb.sb("x1Tb%d" % i, [128, 8, 128], BF16) for i in range(4)]
        x1Tf = [kb.sb("x1Tf%d" % i, [128, 8, 128], F32) for i in range(4)]
        st = [kb.sb("stD%d" % i, [128, 8], F32) for i in range(4)]
        L16 = kb.sb("L16", [128, 16, 36], F32)
        m16 = kb.sb("m16", [128, 16], F32)
        wg16 = kb.sb("wg16", [128, 16], F32)
        m116 = kb.sb("m116", [128, 16], F32)
        m216 = kb.sb("m216", [128, 16], F32)
        e416 = kb.sb("e416", [128, 16, 4], F32)
        gm16 = kb.sb("gm16", [128, 16, 4], F32)
        lem16 = kb.sb("lem16", [128, 16, 32], F32)
        lem216 = kb.sb("lem216", [128, 16, 32], F32)
        mk116 = kb.sb("mk116", [128, 16, 32], F32)
        psD = [kb.ps("psD%d" % i, [128, 512]) for i in range(4)]
        psT = [kb.ps("psT%d" % i, [128, 512]) for i in range(3)]
        psR = kb.ps("psR", [128, 512])
        kb.dma("pool", Wo[:], w_o.rearrange("(c p) f -> p c f", p=128), w=["Wo"])
        kb.dma("sp", Wr[:], router.rearrange("(c p) f -> p c f", p=128), w=["Wr"])
        kb.dma("sp", rbb[:], router_b.partition_broadcast(128), w=["rbb"])
        kb.dma("sp", g1b[:], ln1_g.partition_broadcast(128), w=["g1b"])
        kb.dma("act", b1b[:], ln1_b.partition_broadcast(128), w=["b1b"])
        BIG = 1.0e4
        def tileD(t):
            i = t % 4
            I2 = "%d" % i
            tsl = slice(t * 128, (t + 1) * 128)
            kb.dma(q(), xt[i][:], x[b, tsl, :], w=["xtD" + I2])
            for dh in range(2):
                ps, pk = psD[(2 * t + dh) % 4], "psD%d" % ((2 * t + dh) % 4)
                for c in range(8):
                    kb.mm(ps[:], mixT[:, c, tsl], Wo[:, c, dh * 512:(dh + 1) * 512], start=(c == 0), stop=(c == 7),
                          r=["mixT", "Wo"], w=[pk])
                kb.stt("dve", yt[i][:, dh * 512:(dh + 1) * 512], xt[i][:, dh * 512:(dh + 1) * 512], ALPHA, ps[:], ALU.mult, ALU.add,
                       r=[pk, "xtD" + I2], w=["ytD" + I2])
            s_ = st[i]
            kb.op("dve", lambda e, i=i, s_=s_: e.reduce_sum(s_[:, 0:1], yt[i][:], AX.X), r=["ytD" + I2], w=["stD" + I2])
            yield
            kb.ts("dve", s_[:, 1:2], s_[:, 0:1], -1.0 / D, None, ALU.mult, r=["stD" + I2], w=["stD" + I2])
            kb.ts("dve", yt[i][:], yt[i][:], s_[:, 1:2], None, ALU.add, r=["ytD" + I2, "stD" + I2], w=["ytD" + I2])
            kb.tt("pool", sqt2[i][:], yt[i][:], yt[i][:], ALU.mult, r=["ytD" + I2], w=["sqD" + I2])
            yield
            kb.op("dve", lambda e, s_=s_, q_=sqt2[i]: e.reduce_sum(s_[:, 2:3], q_[:], AX.X), r=["sqD" + I2], w=["stD" + I2])
            kb.ts("dve", s_[:, 3:4], s_[:, 2:3], 1.0 / D, LN_EPS, ALU.mult, ALU.add, r=["stD" + I2], w=["stD" + I2])
            kb.act(s_[:, 4:5], s_[:, 3:4], AF.Sqrt, r=["stD" + I2], w=["stD" + I2])
            yield
            kb.op("dve", lambda e, s_=s_: e.reciprocal(s_[:, 5:6], s_[:, 4:5]), r=["stD" + I2], w=["stD" + I2])
            kb.stt("dve", yt[i][:], yt[i][:], s_[:, 5:6], g1b[:], ALU.mult, ALU.mult, r=["ytD" + I2, "stD" + I2, "g1b"], w=["ytD" + I2])
            kb.tt("pool", yt[i][:], yt[i][:], b1b[:], ALU.add, r=["ytD" + I2, "b1b"], w=["ytD" + I2])
            yield
            kb.dma(q(), x1D[b, tsl, :], yt[i][:], r=["ytD" + I2], w=["x1D"])
            if dbg and dbg[0] == "x1" and b == 0:
                kb.dma("sp", dbg_out[tsl, :], yt[i][:], r=["ytD" + I2], w=["dbgout"])
            for half in range(2):
                ps, pk = psT[(2 * t + half) % 3], "psT%d" % ((2 * t + half) % 3)
                for cc in range(4):
                    c = half * 4 + cc
                    kb.tr(ps[:, cc * 128:(cc + 1) * 128], yt[i][:, c * 128:(c + 1) * 128], identF[:], r=["ytD" + I2, "identF"], w=[pk])
                kb.cp("act", x1Tb[i][:, half * 4:half * 4 + 4, :], ps[:].rearrange("p (c t) -> p c t", t=128), r=[pk], w=["x1Tb" + I2])
                kb.cp("dve", x1Tf[i][:, half * 4:half * 4 + 4, :], ps[:].rearrange("p (c t) -> p c t", t=128), r=[pk], w=["x1Tf" + I2])
            kb.dma(q(), x1TD[b].rearrange("(c p) t -> p c t", p=128)[:, :, tsl], x1Tb[i][:], r=["x1Tb" + I2], w=["x1TD"])
            yield
            for c in range(8):
                kb.mm(psR[:, 0:36], x1Tf[i][:, c, :], Wr[:, c, :], start=(c == 0), stop=(c == 7), r=["x1Tf" + I2, "Wr"], w=["psR"])
            kb.tt("dve", L16[:, t, :], psR[:, 0:36], rbb[:], ALU.add, r=["psR", "rbb"], w=["L16"])

        def run_rr(gens, ways):
            active = [None] * ways
            gens = list(gens)
            while gens or any(g is not None for g in active):
                started = False
                for sl in range(ways):
                    if active[sl] is None and gens and not started:
                        active[sl] = gens.pop(0)
                        started = True
                    if active[sl] is not None:
                        try:
                            next(active[sl])
                        except StopIteration:
                            active[sl] = None

        run_rr([tileD(t) for t in range(16)], 4)
        RK = "rt16"
        bc = lambda ap, n: ap.unsqueeze(2).to_broadcast([128, 16, n])
        lg16 = L16[:, :, 0:4]
        kb.op("dve", lambda e: e.reduce_max(m16[:], lg16, AX.X), r=["L16"], w=[RK])
        kb.tt("dve", e416[:], lg16, bc(m16[:], 4), ALU.subtract, r=["L16", RK], w=[RK])
        kb.act(e416[:], e416[:], AF.Exp, r=[RK], w=[RK])
        kb.op("dve", lambda e: e.reduce_sum(wg16[:], e416[:], AX.X), r=[RK], w=[RK])
        kb.op("dve", lambda e: e.reciprocal(wg16[:], wg16[:]), r=[RK], w=[RK])
        kb.tt("dve", gm16[:], lg16, bc(m16[:], 4), ALU.is_equal, r=["L16", RK], w=[RK])
        kb.ts("dve", gm16[:], gm16[:], BIG, -BIG, ALU.mult, ALU.add, r=[RK], w=[RK])
        for g_ in range(4):
            kb.tt("dve", lem16[:, :, g_ * 8:(g_ + 1) * 8], L16[:, :, 4 + g_ * 8:12 + g_ * 8], bc(gm16[:, :, g_], 8), ALU.add,
                  r=["L16", RK], w=[RK])
        kb.op("dve", lambda e: e.reduce_max(m116[:], lem16[:], AX.X), r=[RK], w=[RK])
        kb.tt("dve", mk116[:], lem16[:], bc(m116[:], 32), ALU.is_equal, r=[RK], w=[RK])
        kb.stt("dve", lem216[:], mk116[:], -BIG, lem16[:], ALU.mult, ALU.add, r=[RK], w=[RK])
        kb.op("dve", lambda e: e.reduce_max(m216[:], lem216[:], AX.X), r=[RK], w=[RK])
        kb.tt("dve", lem16[:], lem216[:], bc(m216[:], 32), ALU.is_equal, r=[RK], w=[RK])
        kb.tt("dve", m216[:], m216[:], m116[:], ALU.subtract, r=[RK], w=[RK])
        kb.act(m216[:], m216[:], AF.Exp, r=[RK], w=[RK])
        kb.ts("dve", m116[:], m216[:], 1.0, None, ALU.add, r=[RK], w=[RK])
        kb.op("dve", lambda e: e.reciprocal(m116[:], m116[:]), r=[RK], w=[RK])
        kb.tt("dve", m216[:], m216[:], m116[:], ALU.mult, r=[RK], w=[RK])
        kb.tt("dve", m116[:], m116[:], wg16[:], ALU.mult, r=[RK], w=[RK])
        kb.tt("dve", m216[:], m216[:], wg16[:], ALU.mult, r=[RK], w=[RK])
        kb.tt("dve", mk116[:], mk116[:], bc(m116[:], 32), ALU.mult, r=[RK], w=[RK])
        kb.tt("dve", lem16[:], lem16[:], bc(m216[:], 32), ALU.mult, r=[RK], w=[RK])
        kb.tt("dve", mk116[:], mk116[:], lem16[:], ALU.add, r=[RK], w=[RK])
        kb.dma("sp", gateD[b].rearrange("(t p) e -> p t e", p=128), mk116[:], r=[RK], w=["gateD"])
        kb.phase_end()
        bes.close()
        bes = ExitStack()
        if stop_after == "D":
            break

        acc = bes.enter_context(nc.sbuf_tensor("acc_b%d" % b, [128, 16, D], F32))
        x1T = bes.enter_context(nc.sbuf_tensor("x1T_b%d" % b, [128, 8, S], BF16))
        gate = bes.enter_context(nc.sbuf_tensor("gate_b%d" % b, [128, 16, 32], F32))
        kb.phase_begin()
        wgu = [kb.sb("wgu%d" % i, [128, 8, 1024], BF16) for i in range(2)]
        wdn = [kb.sb("wdn%d" % i, [128, 4, D], BF16) for i in range(2)]
        hTe = kb.sb("hTe", [128, 4, S], BF16)
        sgt = [kb.sb("sgt%d" % i, [128, 512], BF16) for i in range(2)]
        psG = [kb.ps("psG%d" % i, [128, 512]) for i in range(2)]
        psU = [kb.ps("psU%d" % i, [128, 512]) for i in range(2)]
        psY = [kb.ps("psY%d" % i, [128, 512]) for i in range(4)]
        kb.dma("sp", x1T[:], x1TD[b].rearrange("(c p) t -> p c t", p=128), r=["x1TD"], w=["x1T"])
        kb.dma("act", gate[:], gateD[b].rearrange("(t p) e -> p t e", p=128), r=["gateD"], w=["gate"])
        for t in range(16):
            kb.dma(q(), acc[:, t, :], x1D[b, t * 128:(t + 1) * 128, :], r=["x1D"], w=["acc%d" % t])
            kb.ts("pool", acc[:, t, :], acc[:, t, :], ALPHA, None, ALU.mult, r=["acc%d" % t], w=["acc%d" % t])
        NEXP = int(os.environ.get("DBG_NEXP", "32"))
        ui = 0
        for e_ in range(NEXP):
            i = e_ % 2
            I2 = "%d" % i
            kb.dma("pool", wgu[i][:, :, 0:512], w_gate[e_].rearrange("(c p) f -> p c f", p=128), w=["wgu" + I2])
            kb.dma("pool", wgu[i][:, :, 512:1024], w_up[e_].rearrange("(c p) f -> p c f", p=128), w=["wgu" + I2])
            kb.dma("pool", wdn[i][:], w_down[e_].rearrange("(c p) f -> p c f", p=128), w=["wdn" + I2])
            for tq in range(4):
                for fc in range(4):
                    j = ui % 2
                    ui += 1
                    J2 = "%d" % j
                    for c in range(8):
                        kb.mm(psG[j][:], wgu[i][:, c, fc * 128:(fc + 1) * 128], x1T[:, c, tq * 512:(tq + 1) * 512],
                              start=(c == 0), stop=(c == 7), r=["wgu" + I2, "x1T"], w=["psG" + J2])
                    for c in range(8):
                        kb.mm(psU[j][:], wgu[i][:, c, 512 + fc * 128:512 + (fc + 1) * 128], x1T[:, c, tq * 512:(tq + 1) * 512],
                              start=(c == 0), stop=(c == 7), r=["wgu" + I2, "x1T"], w=["psU" + J2])
                    kb.act(sgt[j][:], psG[j][:], AF.Silu, r=["psG" + J2], w=["sgt" + J2])
                    kb.tt("dve", hTe[:, fc, tq * 512:(tq + 1) * 512], sgt[j][:], psU[j][:], ALU.mult,
                          r=["sgt" + J2, "psU" + J2], w=["hTe%d" % tq])
            for t in range(16):
                for dh in range(2):
                    k = (2 * t + dh) % 4
                    for fc in range(4):
                        kb.mm(psY[k][:], hTe[:, fc, t * 128:(t + 1) * 128], wdn[i][:, fc, dh * 512:(dh + 1) * 512],
                              start=(fc == 0), stop=(fc == 3), r=["hTe%d" % (t // 4), "wdn" + I2], w=["psY%d" % k])
                    kb.stt("dve", acc[:, t, dh * 512:(dh + 1) * 512], psY[k][:], gate[:, t, e_:e_ + 1], acc[:, t, dh * 512:(dh + 1) * 512],
                           ALU.mult, ALU.add, r=["psY%d" % k, "gate", "acc%d" % t], w=["acc%d" % t])
        if dbg and dbg[0] == "accmoe" and b == 0:
            for t in range(16):
                kb.dma("sp", dbg_out[t * 128:(t + 1) * 128, :], acc[:, t, :], r=["acc%d" % t], w=["dbgout"])
        kb.phase_end()
        if stop_after == "E1":
            bes.close()
            break
        kb.phase_begin()
        Gw = kb.sb("Gw", [128, 8, D], BF16)
        Pw = kb.sb("Pw", [128, 2, D], BF16)
        g2b = kb.sb("g2b", [128, D], F32)
        b2b = kb.sb("b2b", [128, D], F32)
        pt_ = [kb.sb("ptE%d" % i, [128, 256], F32) for i in range(4)]
        pTt = [kb.sb("pTt%d" % i, [128, 2, 128], BF16) for i in range(4)]
        sig = [kb.sb("sigE%d" % i, [128, 512], F32) for i in range(2)]
        sqt3 = [kb.sb("sqE%d" % i, [128, D], F32) for i in range(4)]
        st = [kb.sb("stE%d" % i, [128, 8], F32) for i in range(4)]
        psA_ = [kb.ps("psEa%d" % i, [128, 512]) for i in range(2)]
        psB_ = [kb.ps("psEb%d" % i, [128, 512]) for i in range(2)]
        psC_ = [kb.ps("psEc%d" % i, [128, 512]) for i in range(2)]
        kb.dma("pool", Gw[:], ple_gate.rearrange("(c p) f -> p c f", p=128), w=["Gw"])
        kb.dma("pool", Pw[:], ple_proj.rearrange("(c p) f -> p c f", p=128), w=["Pw"])
        kb.dma("sp", g2b[:], ln2_g.partition_broadcast(128), w=["g2b"])
        kb.dma("act", b2b[:], ln2_b.partition_broadcast(128), w=["b2b"])
        def tileE(t):
            i = t % 4
            I2 = "%d" % i
            tsl = slice(t * 128, (t + 1) * 128)
            kb.dma(q(), pt_[i][:], pin[b, tsl, :], w=["ptE" + I2])
            for cc in range(2):
                kb.tr(psC_[i % 2][:, cc * 128:(cc + 1) * 128], pt_[i][:, cc * 128:(cc + 1) * 128], identF[:], r=["ptE" + I2, "identF"], w=["psEc%d" % (i % 2)])
            kb.cp("act", pTt[i][:], psC_[i % 2][:, 0:256].rearrange("p (c t) -> p c t", t=128), r=["psEc%d" % (i % 2)], w=["pTt" + I2])
            yield
            for dh in range(2):
                k = (2 * t + dh) % 2
                K2 = "%d" % k
                dsl = slice(dh * 512, (dh + 1) * 512)
                for c in range(8):
                    kb.mm(psA_[k][:], x1T[:, c, tsl], Gw[:, c, dsl], start=(c == 0), stop=(c == 7), r=["x1T", "Gw"], w=["psEa" + K2])
                kb.act(sig[k][:], psA_[k][:], AF.Sigmoid, r=["psEa" + K2], w=["sigE" + K2])
                for c in range(2):
                    kb.mm(psB_[k][:], pTt[i][:, c, :], Pw[:, c, dsl], start=(c == 0), stop=(c == 1), r=["pTt" + I2, "Pw"], w=["psEb" + K2])
                kb.tt("dve", sig[k][:], sig[k][:], psB_[k][:], ALU.mult, r=["sigE" + K2, "psEb" + K2], w=["sigE" + K2])
                kb.tt("dve", acc[:, t, dsl], acc[:, t, dsl], sig[k][:], ALU.add, r=["sigE" + K2, "acc%d" % t], w=["acc%d" % t])
            y_ = acc[:, t, :]
            AK = "acc%d" % t
            s_ = st[i]
            kb.op("dve", lambda e, y_=y_, s_=s_: e.reduce_sum(s_[:, 0:1], y_, AX.X), r=[AK], w=["stE" + I2])
            yield
            kb.ts("dve", s_[:, 1:2], s_[:, 0:1], -1.0 / D, None, ALU.mult, r=["stE" + I2], w=["stE" + I2])
            kb.ts("dve", y_, y_, s_[:, 1:2], None, ALU.add, r=[AK, "stE" + I2], w=[AK])
            kb.tt("pool", sqt3[i][:], y_, y_, ALU.mult, r=[AK], w=["sqE" + I2])
            yield
            kb.op("dve", lambda e, s_=s_, q_=sqt3[i]: e.reduce_sum(s_[:, 2:3], q_[:], AX.X), r=["sqE" + I2], w=["stE" + I2])
            kb.ts("dve", s_[:, 3:4], s_[:, 2:3], 1.0 / D, LN_EPS, ALU.mult, ALU.add, r=["stE" + I2], w=["stE" + I2])
            kb.act(s_[:, 4:5], s_[:, 3:4], AF.Sqrt, r=["stE" + I2], w=["stE" + I2])
            yield
            kb.op("dve", lambda e, s_=s_: e.reciprocal(s_[:, 5:6], s_[:, 4:5]), r=["stE" + I2], w=["stE" + I2])
            kb.stt("dve", y_, y_, s_[:, 5:6], g2b[:], ALU.mult, ALU.mult, r=[AK, "stE" + I2, "g2b"], w=[AK])
            kb.tt("pool", y_, y_, b2b[:], ALU.add, r=[AK, "b2b"], w=[AK])
            kb.dma(q(), out[b, tsl, :], y_, r=[AK], w=["outD"])
        run_rr([tileE(t) for t in range(16)], 4)
        kb.phase_end()
        bes.close()
    kb.close()
    return nc


def host_inputs(inputs):
    f = lambda a: np.ascontiguousarray(np.asarray(a, dtype=np.float32))
    rel_bias = f(inputs["rel_bias"])
    ext = np.concatenate([rel_bias, np.full((1, 8), NEG, np.float32)], 0)
    idx = bias_index_tables()
    biasT = np.ascontiguousarray(np.transpose(ext[idx], (0, 3, 1, 2)))
    shared = {
        "w_in": f(inputs["w_in"][0]), "mu_rkv": f(inputs["mu_rkv"][0]).reshape(-1), "mu_lora": f(inputs["mu_lora"][0]),
        "w0": f(inputs["w0"][0]), "a0": f(inputs["a0"][0]),
        "w_lora1": f(inputs["w_lora1"][0]), "w_lora2": f(inputs["w_lora2"][0]),
        "a_lora1": f(inputs["a_lora1"][0]), "a_lora2": f(inputs["a_lora2"][0]),
        "g_lora1": f(inputs["g_lora1"][0]), "g_lora2": f(inputs["g_lora2"][0]),
        "k_k": f(inputs["k_k"][0]), "k_a": f(inputs["k_a"][0]), "r_k": f(inputs["r_k"][0]).reshape(-1),
        "lnx_g": f(inputs["lnx_g"][0]), "lnx_b": f(inputs["lnx_b"][0]),
        "biasT": biasT, "w_o": f(inputs["w_o"][0]), "ln1_g": f(inputs["ln1_g"][0]), "ln1_b": f(inputs["ln1_b"][0]),
        "router": np.ascontiguousarray(np.concatenate([f(inputs["router_g"][0]), f(inputs["router_e"][0])], 1)),
        "router_b": np.ascontiguousarray(np.concatenate([f(inputs["router_g_b"][0]), f(inputs["router_e_b"][0])], 0)),
        "w_gate": f(inputs["w_gate"][0]), "w_up": f(inputs["w_up"][0]), "w_down": f(inputs["w_down"][0]),
        "ple_gate": f(inputs["ple_gate"][0]), "ple_proj": f(inputs["ple_proj"][0]),
        "ln2_g": f(inputs["ln2_g"][0]), "ln2_b": f(inputs["ln2_b"][0]),
    }
    return shared


def kernel(**inputs):
    shared = host_inputs(inputs)
    x = np.asarray(inputs["x"], dtype=np.float32)
    p = np.asarray(inputs["p"], dtype=np.float32)[0]
    nc = build_program()
    in_maps = []
    for c in range(8):
        m = dict(shared)
        m["x"] = np.ascontiguousarray(x[c * NB:(c + 1) * NB])
        m["p"] = np.ascontiguousarray(p[c * NB:(c + 1) * NB])
        in_maps.append(m)
    res = run_bass_kernel_spmd(nc, in_maps, core_ids=list(range(8)))
    return np.concatenate([r["out"] for r in res.results], axis=0).astype(np.float32)
```
